# Optimizing a Trainium2 kernel written in Bass

```python
import math
import jax, jax.numpy as jnp
from jax import lax
import numpy as np

D_MODEL = 1024
BATCH = 8
SEQ = 4096
DEPTH = 1

POOL_WINDOWS = (2, 4, 8, 16)
POOL_GROUPS = 4
POOL_GROUP_DIM = 128
POOL_WIDTH = POOL_GROUPS * POOL_GROUP_DIM
ATTN_HEADS = 8
ATTN_HEAD_DIM = 128
ATTN_WIDTH = ATTN_HEADS * ATTN_HEAD_DIM
KV_DIM = ATTN_HEAD_DIM
IDX_HEADS = 4
IDX_DIM = 64
TOPK_MAX = 256
Q_BLOCK = 128
N_BUCKETS = 32
MAX_DISTANCE = 128
N_MEM = 256
MEM_HEADS = 4
MEM_HEAD_DIM = 128
MEM_WIDTH = MEM_HEADS * MEM_HEAD_DIM
D_FF = -(-8 * D_MODEL // (3 * 256)) * 256
EPS = 1e-6

IN_SPLITS = (POOL_WIDTH, ATTN_WIDTH, KV_DIM, KV_DIM, IDX_HEADS * IDX_DIM, IDX_DIM, IDX_HEADS, D_MODEL, D_MODEL)
IN_WIDTH = sum(IN_SPLITS)

kernel_name = "hybrid_pool_dsa_gated_block"


def _rmsnorm(x, g):
    xf = x.astype(jnp.float32)
    y = xf * lax.rsqrt(jnp.mean(xf * xf, axis=-1, keepdims=True) + EPS)
    return (y * g.astype(jnp.float32)).astype(x.dtype)


def _t5_bucket(dist):
    n = jnp.maximum(dist, 0)
    max_exact = N_BUCKETS // 2
    nf = jnp.maximum(n, 1).astype(jnp.float32)
    large = max_exact + (jnp.log(nf / max_exact) / math.log(MAX_DISTANCE / max_exact)
                         * (N_BUCKETS - max_exact)).astype(jnp.int32)
    large = jnp.minimum(large, N_BUCKETS - 1)
    return jnp.where(n < max_exact, n, large)


def _pool_mixer(u, pool_w, pool_scale):
    B, S, _ = u.shape
    ug = u.reshape(B, S, POOL_GROUPS, POOL_GROUP_DIM).astype(jnp.float32)
    cs = jnp.concatenate([jnp.zeros((B, 1, POOL_GROUPS, POOL_GROUP_DIM), jnp.float32),
                          jnp.cumsum(ug, axis=1)], axis=1)
    win = jnp.array(POOL_WINDOWS, jnp.int32)
    t1 = jnp.arange(1, S + 1, dtype=jnp.int32)[:, None]
    lo = jnp.maximum(t1 - win[None, :], 0)
    cnt = jnp.minimum(t1, win[None, :]).astype(jnp.float32)
    g_idx = jnp.arange(POOL_GROUPS)[None, :]
    mean = (cs[:, 1:] - cs[:, lo, g_idx, :]) / cnt[None, :, :, None]
    pooled = (mean - ug).astype(u.dtype)
    mixed = jnp.einsum('bsgc,gcd->bsgd', pooled, pool_w)
    return mixed.reshape(B, S, POOL_WIDTH) * pool_scale


def _sparse_attention(q, k, v, q_idx, k_idx, w_idx, rel_bias_table):
    B, S = q.shape[:2]
    n_sel = min(TOPK_MAX, S // 4)
    nb = S // Q_BLOCK
    key_pos = jnp.arange(S, dtype=jnp.int32)
    idx_scale = IDX_DIM ** -0.5
    w_scale = IDX_HEADS ** -0.5
    attn_scale = ATTN_HEAD_DIM ** -0.5

    def blockify(a):
        return a.reshape((B, nb, Q_BLOCK) + a.shape[2:]).swapaxes(0, 1)

    def one_block(args):
        qb, qib, wb, start = args
        q_pos = start + jnp.arange(Q_BLOCK, dtype=jnp.int32)
        dots = jnp.einsum('bthd,bsd->bths', qib, k_idx).astype(jnp.float32) * idx_scale
        score = jnp.einsum('bths,bth->bts', jax.nn.relu(dots), wb.astype(jnp.float32) * w_scale)
        causal = key_pos[None, :] <= q_pos[:, None]
        score = jnp.where(causal[None], score, -jnp.inf)
        _, sel = lax.top_k(score, n_sel)
        valid = sel <= q_pos[None, :, None]
        kg = jax.vmap(lambda kb, ib: kb[ib])(k, sel)
        vg = jax.vmap(lambda vb, ib: vb[ib])(v, sel)
        logits = jnp.einsum('bthd,btkd->bhtk', qb, kg).astype(jnp.float32) * attn_scale
        bias = rel_bias_table[_t5_bucket(q_pos[None, :, None] - sel)]
        logits = logits + jnp.moveaxis(bias, -1, 1).astype(jnp.float32)
        logits = jnp.where(valid[:, None], logits, -jnp.inf)
        p = jax.nn.softmax(logits, axis=-1).astype(vg.dtype)
        return jnp.einsum('bhtk,btkd->bthd', p, vg)

    starts = jnp.arange(nb, dtype=jnp.int32) * Q_BLOCK
    out = lax.map(one_block, (blockify(q), blockify(q_idx), blockify(w_idx), starts))
    return out.swapaxes(0, 1).reshape(B, S, ATTN_HEADS * ATTN_HEAD_DIM)


def _mixer_sublayer(h, w_in, pool_w, pool_scale, w_proj_pool, w_proj_attn, rel_bias_table, w_out):
    B, S, _ = h.shape
    proj = h @ w_in
    offs = np.cumsum(IN_SPLITS)[:-1].tolist()
    u_pool, q, k, v, q_idx, k_idx, w_idx, g_pool, g_attn = jnp.split(proj, offs, axis=-1)
    a = _pool_mixer(u_pool, pool_w, pool_scale) @ w_proj_pool
    att = _sparse_attention(q.reshape(B, S, ATTN_HEADS, ATTN_HEAD_DIM), k, v,
                            q_idx.reshape(B, S, IDX_HEADS, IDX_DIM), k_idx, w_idx,
                            rel_bias_table)
    b = att @ w_proj_attn
    merged = jax.nn.sigmoid(g_pool) * a + jax.nn.sigmoid(g_attn) * b
    return merged @ w_out


def _memory_cross_attention(h, mem, norm_mem_kv, w_q_mem, w_kv_mem, w_o_mem):
    B, S, _ = h.shape
    q = (h @ w_q_mem).reshape(B, S, MEM_HEADS, MEM_HEAD_DIM)
    kv = _rmsnorm(mem, norm_mem_kv) @ w_kv_mem
    k, v = jnp.split(kv, 2, axis=-1)
    k = k.reshape(B, N_MEM, MEM_HEADS, MEM_HEAD_DIM)
    v = v.reshape(B, N_MEM, MEM_HEADS, MEM_HEAD_DIM)
    logits = jnp.einsum('bshd,bmhd->bhsm', q, k).astype(jnp.float32) * MEM_HEAD_DIM ** -0.5
    p = jax.nn.softmax(logits, axis=-1).astype(v.dtype)
    o = jnp.einsum('bhsm,bmhd->bshd', p, v).reshape(B, S, MEM_WIDTH)
    return o @ w_o_mem


def _swiglu(h, w_gate_up, w_down):
    g, u = jnp.split(h @ w_gate_up, 2, axis=-1)
    return (jax.nn.silu(g) * u) @ w_down


def setup_inputs(seed: int = 0) -> dict:
    key = jax.random.key(seed)
    ks = jax.random.split(key, 24)
    f32 = jnp.float32

    def w(k, shape, fan_in):
        return jax.random.normal(k, shape, f32) * fan_in ** -0.5

    def gain(k, n):
        return 1.0 + 0.02 * jax.random.normal(k, (n,), f32)

    return {
        "x": jax.random.normal(ks[0], (BATCH, SEQ, D_MODEL), f32),
        "mem": jax.random.normal(ks[1], (BATCH, N_MEM, D_MODEL), f32),
        "norm_mix_pre": gain(ks[2], D_MODEL),
        "norm_mix_post": gain(ks[3], D_MODEL),
        "w_in": w(ks[4], (D_MODEL, IN_WIDTH), D_MODEL),
        "pool_w": w(ks[5], (POOL_GROUPS, POOL_GROUP_DIM, POOL_GROUP_DIM), POOL_GROUP_DIM),
        "pool_scale": gain(ks[6], POOL_WIDTH),
        "w_proj_pool": w(ks[7], (POOL_WIDTH, D_MODEL), POOL_WIDTH),
        "w_proj_attn": w(ks[8], (ATTN_WIDTH, D_MODEL), ATTN_WIDTH),
        "rel_bias_table": 0.5 * jax.random.normal(ks[9], (N_BUCKETS, ATTN_HEADS), f32),
        "w_out": w(ks[10], (D_MODEL, D_MODEL), D_MODEL),
        "norm_mem_pre": gain(ks[11], D_MODEL),
        "norm_mem_kv": gain(ks[12], D_MODEL),
        "norm_mem_post": gain(ks[13], D_MODEL),
        "w_q_mem": w(ks[14], (D_MODEL, MEM_WIDTH), D_MODEL),
        "w_kv_mem": w(ks[15], (D_MODEL, 2 * MEM_WIDTH), D_MODEL),
        "w_o_mem": w(ks[16], (MEM_WIDTH, D_MODEL), MEM_WIDTH),
        "norm_ffn_pre": gain(ks[17], D_MODEL),
        "norm_ffn_post": gain(ks[18], D_MODEL),
        "w_gate_up": w(ks[19], (D_MODEL, 2 * D_FF), D_MODEL),
        "w_down": w(ks[20], (D_FF, D_MODEL), D_FF),
    }


def reference(x, mem, norm_mix_pre, norm_mix_post, w_in, pool_w, pool_scale, w_proj_pool,
              w_proj_attn, rel_bias_table, w_out, norm_mem_pre, norm_mem_kv, norm_mem_post,
              w_q_mem, w_kv_mem, w_o_mem, norm_ffn_pre, norm_ffn_post, w_gate_up, w_down):
    for _ in range(DEPTH):
        h = _rmsnorm(x, norm_mix_pre)
        x = x + _rmsnorm(_mixer_sublayer(h, w_in, pool_w, pool_scale, w_proj_pool,
                                         w_proj_attn, rel_bias_table, w_out), norm_mix_post)
        h = _rmsnorm(x, norm_mem_pre)
        x = x + _rmsnorm(_memory_cross_attention(h, mem, norm_mem_kv, w_q_mem, w_kv_mem, w_o_mem),
                         norm_mem_post)
        h = _rmsnorm(x, norm_ffn_pre)
        x = x + _rmsnorm(_swiglu(h, w_gate_up, w_down), norm_ffn_post)
    return x
```

```python
import numpy as np
import concourse.bass as bass
import concourse.mybir as mybir
from concourse.bass_utils import run_bass_kernel_spmd

F32 = mybir.dt.float32
BF16 = mybir.dt.bfloat16
ALU = mybir.AluOpType
AF = mybir.ActivationFunctionType
AX = mybir.AxisListType

S = 4096
D = 1024
NT = S // 128
DFF = 2816
NF = DFF // 128
NMEM = 256
EPS = 1e-6
IN_W = 4164
OFF_U, OFF_Q, OFF_K, OFF_V, OFF_QI, OFF_KI, OFF_WI, OFF_GP, OFF_GA = 0, 512, 1536, 1664, 1792, 2048, 2112, 2116, 3140
TOPK = 256
NBISECT = 20
BIG = 30000.0

ENGS = ("pe", "act", "dve", "pool", "sp")
SEM_EPOCH = 30000


class Op:
    __slots__ = ("eng", "fn", "dma", "semkey", "deps", "sig", "needed", "idx")

    def __init__(self, eng, fn, dma, semkey):
        self.eng = eng
        self.fn = fn
        self.dma = dma
        self.semkey = semkey
        self.deps = {}
        self.sig = None
        self.needed = False


class Prog:
    def __init__(self, nc):
        self.nc = nc
        self.ops = []
        self.lastw = {}
        self.readers = {}
        self.sems = {}
        self.pass_start = 0
        self.cnt = {e: 0 for e in ENGS}
        self.dcnt = {}
        self.waited = {e: {} for e in ENGS}

    def add(self, eng, fn, r=(), w=(), dma=False, semkey=None):
        op = Op(eng, fn, dma, semkey)
        op.idx = len(self.ops)
        for k in r:
            lw = self.lastw.get(k)
            if lw is not None:
                op.deps[lw.idx] = (lw, "RAW")
        for k in w:
            lw = self.lastw.get(k)
            if lw is not None:
                op.deps[lw.idx] = (lw, "WAW")
            for rd in self.readers.get(k, ()):
                if rd.idx not in op.deps:
                    op.deps[rd.idx] = (rd, "WAR")
        for k in r:
            self.readers.setdefault(k, []).append(op)
        for k in w:
            self.lastw[k] = op
            self.readers[k] = []
        self.ops.append(op)
        return op

    def _sem(self, name):
        s = self.sems.get(name)
        if s is None:
            s = self.nc.alloc_semaphore(name)
            self.sems[name] = s
        return s

    def barrier(self):
        last = {}
        for op in self.ops[self.pass_start:]:
            last[op.eng] = op
            if op.dma:
                last["dma_" + op.semkey] = op
        fences = list(last.values())
        for e in ENGS:
            op = Op(e, None, False, None)
            op.idx = len(self.ops)
            for f in fences:
                op.deps[f.idx] = (f, "RAW")
            self.ops.append(op)
        self.lastw = {}
        self.readers = {}

    def flush(self, engines):
        ops = self.ops[self.pass_start:]
        self.pass_start = len(self.ops)
        for op in ops:
            keep = {}
            for i, (d, kind) in op.deps.items():
                if d is op:
                    continue
                if not d.dma and d.eng == op.eng:
                    if op.eng == "pe" and not op.dma:
                        continue
                keep[i] = (d, kind)
            op.deps = keep
            for d, _ in keep.values():
                d.needed = True
        for op in ops:
            if op.dma:
                c = self.dcnt.get(op.semkey, 0) + 1
                self.dcnt[op.semkey] = c
                op.sig = ("dma_" + op.semkey, 16 * c)
            elif op.needed:
                c = self.cnt[op.eng]
                self.cnt[op.eng] = c + 1
                op.sig = ("e_%s_%d" % (op.eng, c // SEM_EPOCH), c % SEM_EPOCH + 1)
        by = {e: [] for e in ENGS}
        for op in ops:
            by[op.eng].append(op)

        def replay(e, eo):
            waited = self.waited[e]
            for op in by[e]:
                need = {}
                for d, _ in op.deps.values():
                    sn, v = d.sig
                    if need.get(sn, 0) < v:
                        need[sn] = v
                for sn, v in need.items():
                    if waited.get(sn, 0) >= v:
                        continue
                    waited[sn] = v
                    eo.wait_ge(self._sem(sn), v)
                if op.fn is None:
                    continue
                ins = op.fn(eo)
                if op.sig is not None:
                    sn, v = op.sig
                    ins.then_inc(self._sem(sn), 16 if op.dma else 1)
        return replay

    def final_waits(self, eo):
        for k, c in self.dcnt.items():
            eo.wait_ge(self._sem("dma_" + k), 16 * c)


def sb_bcast_free(t, col0, ncol, rep):
    W = t.shape[1]
    return bass.AP(t, col0, [[W, 128], [1, ncol], [0, rep]])


C_IDENT, C_CAUS, C_CMAT, C_OHR, C_POW2, C_INVC = 0, 128, 256, 288, 672, 704
V_MIXPRE, V_MIXPOST, V_MEMPRE, V_MEMKV, V_MEMPOST, V_FFNPRE, V_FFNPOST, V_PSCALE = range(8)


def make_consts():
    c = np.zeros((128, 1024), np.float32)
    c[:, C_IDENT:C_IDENT + 128] = np.eye(128, dtype=np.float32)
    t = np.arange(128)[:, None]
    s = np.arange(128)[None, :]
    c[:, C_CAUS:C_CAUS + 128] = np.where(s <= t, 0.0, -BIG)
    cm = np.eye(32, dtype=np.float32)
    cm[31, :] -= 1.0
    c[0:32, C_CMAT:C_CMAT + 32] = cm
    d = np.maximum(255 - np.arange(383), 0)
    nf = np.maximum(d, 1).astype(np.float32)
    large = 16 + (np.log(nf / np.float32(16)) / np.float32(np.log(128 / 16)) * np.float32(16)).astype(np.int32)
    large = np.minimum(large, 31)
    bucket = np.where(d < 16, d, large)
    bucket = np.where(np.arange(383) > 255, 31, bucket)
    oh = np.zeros((32, 383), np.float32)
    oh[bucket, np.arange(383)] = 1.0
    c[0:32, C_OHR:C_OHR + 383] = oh
    c[:, C_POW2:C_POW2 + 32] = (0.5 ** (np.arange(32) + 1)).astype(np.float32)[None, :]
    for g, w in enumerate((2, 4, 8, 16)):
        c[:, C_INVC + g * 16:C_INVC + (g + 1) * 16] = (1.0 / np.minimum(np.arange(16) + 1, w)).astype(np.float32)[None, :]
    return c


class Builder:
    def __init__(self, stages="ABC", debug=False):
        self.stages = stages
        nc = bass.Bass("TRN2", target_bir_lowering=False)
        self.nc = nc
        self.P = Prog(nc)
        dt = nc.dram_tensor
        ei = "ExternalInput"
        self.x = dt("x", [S, D], F32, kind=ei).ap()
        self.mem = dt("mem", [NMEM, D], F32, kind=ei).ap()
        self.w_in = dt("w_in", [D, IN_W], F32, kind=ei).ap()
        self.pool_w = dt("pool_w", [4, 128, 128], F32, kind=ei).ap()
        self.w_proj_pool = dt("w_proj_pool", [512, D], F32, kind=ei).ap()
        self.w_proj_attn = dt("w_proj_attn", [D, D], F32, kind=ei).ap()
        self.rel_bias = dt("rel_bias_table", [32, 8], F32, kind=ei).ap()
        self.w_out = dt("w_out", [D, D], F32, kind=ei).ap()
        self.w_q_mem = dt("w_q_mem", [D, 512], F32, kind=ei).ap()
        self.w_kv_mem = dt("w_kv_mem", [D, D], F32, kind=ei).ap()
        self.w_o_mem = dt("w_o_mem", [512, D], F32, kind=ei).ap()
        self.w_gate_up = dt("w_gate_up", [D, 2 * DFF], F32, kind=ei).ap()
        self.w_down = dt("w_down", [DFF, D], F32, kind=ei).ap()
        self.vecs_t = dt("vecs", [8, D], F32, kind=ei)
        self.vecs = self.vecs_t.ap()
        self.vecsT = dt("vecsT", [128, 64], F32, kind=ei).ap()
        self.consts = dt("consts", [128, 1024], F32, kind=ei).ap()
        self.out = dt("out", [S, D], F32, kind="ExternalOutput").ap()

        def scratch(name, shape, dtype, producer):
            if not debug:
                kind = "Internal"
            else:
                kind = "ExternalOutput" if producer in stages else ei
            return dt(name, shape, dtype, kind=kind).ap()
        self.PMd = scratch("PMd", [4, 128, S], BF16, "A")
        self.ATd = scratch("ATd", [8, 128, S], BF16, "A")
        self.X2d = scratch("X2d", [S, D], F32, "B")

    def add(self, *a, **k):
        return self.P.add(*a, **k)

    def dma(self, out, in_, r, w, semkey, eng="sp", **kw):
        return self.P.add(eng, lambda e: e.dma_start(out=out, in_=in_, **kw), r=r, w=w, dma=True, semkey=semkey)

    def mm(self, out, lhsT, rhs, start, stop, r, w, **kw):
        return self.P.add("pe", lambda e: e.matmul(out, lhsT, rhs, start=start, stop=stop, **kw), r=r, w=w)

    def tr(self, out, in_, r, w):
        ident = self.ident
        return self.P.add("pe", lambda e: e.transpose(out, in_, ident), r=tuple(r) + ("consts",), w=w)

    def load_w(self, name, dst, src, nchunk):
        for c in range(nchunk):
            self.dma(dst[:, c, :], src[c * 128:(c + 1) * 128, :], r=(), w=(name,), semkey=name, eng="pool",
                     max_dma_last_dim=4096)

    def run_pass(self, body):
        from contextlib import ExitStack
        nc = self.nc
        with ExitStack() as st:
            self.st = st
            self.pname = body.__name__
            body()
            self.P.barrier()
            last = (body == self.bodies[-1])
            with nc.Block() as block:
                replay = self.P.flush(None)

                @block.tensor
                def _(e):
                    replay("pe", e)

                @block.scalar
                def _(e):
                    replay("act", e)

                @block.vector
                def _(e):
                    replay("dve", e)

                @block.gpsimd
                def _(e):
                    replay("pool", e)

                @block.sync
                def _(e):
                    replay("sp", e)
                    if last:
                        self.P.final_waits(e)

    def sb(self, name, shape, dtype):
        return self.st.enter_context(self.nc.sbuf_tensor("%s_%s" % (self.pname, name), list(shape), dtype)).ap()

    def common_alloc(self):
        nc = self.nc
        self.cst = self.sb("cst", [128, 1024], F32)
        self.ident = self.cst[:, C_IDENT:C_IDENT + 128]
        self.vT = self.sb("vT", [128, 64], F32)
        self.ps = self.st.enter_context(nc.psum_tensor("ps_" + self.pname, [128, 8, 512], F32)).ap()
        self.stat = self.sb("stat", [128, 256], F32)
        self.stat_i = 0
        self.junk = self.sb("junk", [128, 1024], F32)
        if self.pname != "pass_A":
            self.ysb = self.sb("ysb", [128, 1024], F32)
        self.dma(self.cst[:, :], self.consts[:, :], r=(), w=("consts",), semkey="consts")
        self.dma(self.vT[:, :], self.vecsT[:, :], r=(), w=("vT",), semkey="vT")

    def stat_slot(self, n=1):
        i = self.stat_i
        if i + n > 256:
            i = 0
        self.stat_i = i + n
        return self.stat[:, i:i + n], "stat%d" % i

    def gbc_load(self, name, row):
        t = self.sb(name, [128, D], F32)
        src = bass.AP(self.vecs_t, row * D, [[0, 128], [1, D]])
        self.dma(t[:, :], src, r=(), w=(name,), semkey=name)
        return t

    def rstd_of(self, ss, ssk, n):
        ms, msk = self.stat_slot()
        self.add("dve", lambda e: e.tensor_scalar(ms, ss, 1.0 / n, EPS, ALU.mult, ALU.add), r=(ssk,), w=(msk,))
        sd, sdk = self.stat_slot()
        self.add("act", lambda e: e.activation(sd, ms, AF.Sqrt), r=(msk,), w=(sdk,))
        rs, rsk = self.stat_slot()
        self.add("dve", lambda e: e.reciprocal(rs, sd), r=(sdk,), w=(rsk,))
        return rs, rsk

    def norm_to_hT(self, xt, xtk, vrow, hT, hTk, sub, hs, hsk, banks=(6, 7)):
        ss, ssk = self.stat_slot()
        junk = self.junk
        self.add("act", lambda e: e.activation(junk, xt, AF.Square, accum_out=ss), r=(xtk,), w=("junk", ssk))
        rs, rsk = self.rstd_of(ss, ssk, D)
        self.add("dve", lambda e: e.tensor_scalar(hs, xt, rs, None, ALU.mult), r=(xtk, rsk), w=(hsk,))
        for half in range(2):
            b = banks[half]
            pk = "ps%d" % b
            for jj in range(4):
                j = half * 4 + jj
                self.tr(self.ps[:, b, jj * 128:(jj + 1) * 128], hs[:, j * 128:(j + 1) * 128], r=(hsk,), w=(pk,))
            src = self.ps[:, b, :].rearrange("p (j t) -> p j t", j=4)
            g = bass.AP(self.vT.tensor, vrow * 8 + half * 4, [[64, 128], [1, 4], [0, 128]])
            dst = hT[:, half * 4:half * 4 + 4, sub * 128:(sub + 1) * 128]
            self.add("dve", lambda e, dst=dst, src=src, g=g: e.tensor_tensor(dst, src, g, ALU.mult),
                     r=(pk, "vT"), w=(hTk,))

    def post_norm_residual(self, ybanks, gbc, gbck, xt, xtk, tmp, tmpk):
        ysb = self.ysb
        for hf in range(2):
            b = ybanks[hf]
            y = self.ps[:, b, :]
            self.add("act", lambda e, y=y, hf=hf: e.activation(ysb[:, hf * 512:(hf + 1) * 512], y, AF.Copy),
                     r=("ps%d" % b,), w=("ysb",))
        ss, ssk = self.stat_slot()
        junk = self.junk
        self.add("act", lambda e: e.activation(junk, ysb, AF.Square, accum_out=ss), r=("ysb",), w=("junk", ssk))
        self.add("dve", lambda e: e.tensor_tensor(tmp, ysb, gbc, ALU.mult), r=("ysb", gbck), w=(tmpk,))
        rs, rsk = self.rstd_of(ss, ssk, D)
        self.add("dve", lambda e: e.scalar_tensor_tensor(xt, tmp, rs, xt, ALU.mult, ALU.add),
                 r=(tmpk, rsk, xtk), w=(xtk,))

    def pass_C(self):
        self.common_alloc()
        src = self.X2d if "B" in self.stages else self.x
        Wgu = self.sb("Wgu", [128, 8, 2 * DFF], BF16)
        Wd = self.sb("Wd", [128, NF, D], BF16)
        self.load_w("Wgu", Wgu, self.w_gate_up, 8)
        self.load_w("Wd", Wd, self.w_down, NF)
        gbc = self.gbc_load("gbcF", V_FFNPOST)
        xt = [self.sb("xt%d" % i, [128, D], F32) for i in range(4)]
        hs = self.sb("hs", [128, D], F32)
        tmp = self.sb("tmp", [128, D], F32)
        hT = [self.sb("hT%d" % i, [128, 8, 256], BF16) for i in range(2)]
        aT = self.sb("aT", [128, NF, 256], BF16)
        sg = [self.sb("sg%d" % i, [128, 256], F32) for i in range(3)]
        ps = self.ps
        def load_norm(c):
            t0 = c * 256
            for sub in range(2):
                xi = (c % 2) * 2 + sub
                self.dma(xt[xi][:, :], src[t0 + sub * 128:t0 + (sub + 1) * 128, :], r=(), w=("xt%d" % xi,),
                         semkey="xt%d" % xi)
                self.norm_to_hT(xt[xi], "xt%d" % xi, V_FFNPRE, hT[c % 2], "hT%d" % (c % 2), sub, hs, "hs", banks=(3, 3))

        def ffn1(c):
            h = hT[c % 2]
            hk = "hT%d" % (c % 2)
            for j in range(NF):
                b = j % 3
                pk = "ps%d" % b
                for k in range(8):
                    self.mm(ps[:, b, 0:256], Wgu[:, k, j * 128:(j + 1) * 128], h[:, k, :], k == 0, k == 7,
                            r=("Wgu", hk), w=(pk,))
                for k in range(8):
                    self.mm(ps[:, b, 256:512], Wgu[:, k, DFF + j * 128:DFF + (j + 1) * 128], h[:, k, :], k == 0, k == 7,
                            r=("Wgu", hk), w=(pk,))
                s_ = sg[j % 3]
                sk = "sg%d" % (j % 3)
                self.add("act", lambda e, s_=s_, b=b: e.activation(s_, ps[:, b, 0:256], AF.Silu), r=(pk,), w=(sk,))
                self.add("dve", lambda e, s_=s_, b=b, j=j: e.tensor_tensor(aT[:, j, :], s_, ps[:, b, 256:512], ALU.mult),
                         r=(pk, sk), w=("aT",))

        def ffn2(c):
            t0 = c * 256
            for sub in range(2):
                xi = (c % 2) * 2 + sub
                yb = (4, 5) if sub == 0 else (6, 7)
                for half in range(2):
                    pk = "ps%d" % yb[half]
                    for j in range(NF):
                        self.mm(ps[:, yb[half], :], aT[:, j, sub * 128:(sub + 1) * 128], Wd[:, j, half * 512:(half + 1) * 512],
                                j == 0, j == NF - 1, r=("aT", "Wd"), w=(pk,))
                self.post_norm_residual(yb, gbc, "gbcF", xt[xi], "xt%d" % xi, tmp, "tmp")
                self.dma(self.out[t0 + sub * 128:t0 + (sub + 1) * 128, :], xt[xi][:, :], r=("xt%d" % xi,), w=(),
                         semkey="xt%d" % xi)

        NCH = S // 256
        load_norm(0)
        for c in range(NCH):
            ffn1(c)
            if c + 1 < NCH:
                load_norm(c + 1)
            ffn2(c)

    def pass_A(self):
        self.common_alloc()
        ps = self.ps
        cst = self.cst
        NB = NBISECT
        SC = float(128 ** -0.5)
        WA = OFF_GP
        Wa = self.sb("Wa", [128, 8, WA], BF16)
        self.load_w("Wa", Wa, self.w_in[:, 0:WA], 8)
        Wpool = self.sb("Wpool", [128, 4, 128], BF16)
        for g in range(4):
            self.dma(Wpool[:, g, :], self.pool_w[g], r=(), w=("Wpool",), semkey="Wpool", eng="pool")
        KT = self.sb("KT", [128, S], BF16)
        V = self.sb("V", [128, NT, 130], BF16)
        kiT = self.sb("kiT", [128, S], BF16)
        xt = [self.sb("xt%d" % i, [128, D], F32) for i in range(2)]
        hs = self.sb("hs", [128, D], F32)
        hT = self.sb("hT", [128, 8, 256], BF16)
        ubuf = self.sb("ubuf", [128, 4, 272], F32)
        pa = self.sb("pa", [128, 272], F32)
        pb = self.sb("pb", [128, 272], F32)
        t16 = self.sb("t16", [128, 16], F32)
        pooledT = self.sb("pooledT", [128, 4, 256], BF16)
        pmT = [self.sb("pmT%d" % i, [128, 4, 256], BF16) for i in range(2)]
        qTs = [self.sb("qT%d" % i, [128, 8, 256], BF16) for i in range(2)]
        qiT = self.sb("qiT", [128, 2, 256], BF16)
        widx = self.sb("widx", [128, 2, 4], F32)
        diagw = [self.sb("diagw%d" % i, [128, 4, 128], BF16) for i in range(2)]
        identb = self.sb("identb", [128, 128], BF16)
        causb = self.sb("causb", [128, 128], BF16)
        score = [self.sb("score%d" % i, [128, S], F32) for i in range(2)]
        Rb = [self.sb("R%d" % i, [128, 4, 512], BF16) for i in range(2)]
        maskc = [self.sb("maskc%d" % i, [128, 512], F32) for i in range(2)]
        maskTs = [self.sb("maskT%d" % i, [128, NT, 256], BF16) for i in range(2)]
        PT = [self.sb("PT%d" % i, [128, 256], BF16) for i in range(4)]
        PTm = [self.sb("PTm%d" % i, [128, 256], BF16) for i in range(4)]
        att = [self.sb("att%d" % i, [128, D], F32) for i in range(2)]
        atT = [self.sb("atT%d" % i, [128, 8, 256], BF16) for i in range(2)]
        EB = self.sb("EB", [128, 8, 256], BF16)
        tbl = self.sb("tbl", [128, 8], F32)
        tblD = self.sb("tblD", [128, 8], F32)
        HK = [self.sb("HK%d" % i, [128, 32], F32) for i in range(2)]

        self.add("pool", lambda e: e.memset(V, 1.0), w=tuple("V%d" % i for i in range(S // 256)))
        self.add("pool", lambda e: e.memset(ubuf, 0.0), w=tuple("ubuf%d" % g for g in range(4)))
        self.add("dve", lambda e: e.tensor_copy(identb, cst[:, C_IDENT:C_IDENT + 128]), r=("consts",), w=("identb",))
        self.add("dve", lambda e: e.tensor_copy(causb, cst[:, C_CAUS:C_CAUS + 128]), r=("consts",), w=("causb",))
        self.dma(tbl[0:32, :], self.rel_bias[:, :], r=(), w=("tbl",), semkey="tbl")
        self.mm(ps[0:32, 0, 0:8], cst[0:32, C_CMAT:C_CMAT + 32], tbl[0:32, :], True, True, r=("consts", "tbl"), w=("ps0",))
        self.add("act", lambda e: e.activation(tblD[0:32, :], ps[0:32, 0, 0:8], AF.Copy), r=("ps0",), w=("tblD",))
        for jj in range(256):
            b = jj // 64
            self.mm(ps[:, b, (jj % 64) * 8:(jj % 64) * 8 + 8], cst[0:32, C_OHR + 255 - jj:C_OHR + 383 - jj], tblD[0:32, :],
                    True, True, r=("consts", "tblD"), w=("ps%d" % b,))
        for b in range(4):
            src = ps[:, b, :].rearrange("p (j h) -> p h j", h=8)
            self.add("act", lambda e, b=b, src=src: e.activation(EB[:, :, b * 64:(b + 1) * 64], src, AF.Exp),
                     r=("ps%d" % b,), w=("EB",))

        def BK(b):
            return ("ps%d" % b,)

        rot = {"L": 0, "P": 0, "T": 0, "J": 0, "R": 0, "S": 0, "M": 0}

        def nxt(k, n):
            v = rot[k]
            rot[k] = (v + 1) % n
            return v

        BS = {}

        def bsetup(c, sub):
            los, st, kd = BS[c]
            qi = 2 * c + sub
            n = 128 * (qi + 1)
            sk = "score%d" % sub
            if qi < 2:
                lo, lok = self.stat_slot()
                self.add("dve", lambda e, lo=lo: e.memset(lo, -10000.0), w=(lok,))
                los[sub] = (lo, lok)
                return
            mx, mxk = self.stat_slot()
            mn, mnk = self.stat_slot()
            self.add("dve", lambda e: e.tensor_reduce(mx, score[sub][:, 0:n], AX.X, ALU.max), r=(sk,), w=(mxk,))
            self.add("dve", lambda e: e.tensor_reduce(mn, score[sub][:, 0:n - 128], AX.X, ALU.min), r=(sk,), w=(mnk,))
            w0, w0k = self.stat_slot()
            self.add("dve", lambda e: e.scalar_tensor_tensor(w0, mx, 1.0, mn, ALU.add, ALU.subtract), r=(mxk, mnk), w=(w0k,))
            hk = HK[sub]
            hkk = "HK%d" % sub
            self.add("dve", lambda e: e.tensor_scalar(hk, cst[:, C_POW2:C_POW2 + 32], w0, None, ALU.mult), r=("consts", w0k), w=(hkk,))
            mid, midk = self.stat_slot()
            self.add("dve", lambda e: e.tensor_tensor(mid, mn, hk[:, 0:1], ALU.add), r=(mnk, hkk), w=(midk,))
            st[sub] = (mid, midk, hk, hkk, n, sk)

        def F2one(c, sub):
            los, st, kd = BS[c]
            if st[sub] is None or kd[sub] >= NB:
                return False
            k = kd[sub]
            kd[sub] = k + 1
            junkb = maskTs[c % 2].rearrange("p a b -> p (a b)")
            jk = "maskT%d" % (c % 2)
            mid, midk, hk, hkk, n, sk = st[sub]
            cnt, cntk = self.stat_slot()
            self.add("dve", lambda e: e.tensor_scalar(junkb[:, 0:n], score[sub][:, 0:n], mid, None, ALU.is_ge, ALU.add, accum_out=cnt),
                     r=(sk, midk), w=(jk, cntk))
            sg_, sgk = self.stat_slot()
            self.add("dve", lambda e: e.scalar_tensor_tensor(sg_, cnt, TOPK - 0.5, hk[:, k:k + 1], ALU.is_ge, ALU.mult),
                     r=(cntk, hkk), w=(sgk,))
            mid2, mid2k = self.stat_slot()
            self.add("dve", lambda e: e.scalar_tensor_tensor(mid2, mid, hk[:, k + 1:k + 2], sg_, ALU.subtract, ALU.add),
                     r=(midk, hkk, sgk), w=(mid2k,))
            st[sub] = (mid2, mid2k, hk, hkk, n, sk)
            return True

        def F2rem(c):
            los, st, kd = BS[c]
            return sum(NB - kd[s_] for s_ in range(2) if st[s_] is not None)

        def F2some(c, cnt_):
            los, st, kd = BS[c]
            for _ in range(cnt_):
                order = sorted(range(2), key=lambda s_: kd[s_])
                for s_ in order:
                    if F2one(c, s_):
                        break

        def F1(c):
            t0 = c * 256
            qT = qTs[c % 2]
            qTk = "qT%d" % (c % 2)
            for sub in range(2):
                self.dma(xt[sub][:, :], self.x[t0 + sub * 128:t0 + (sub + 1) * 128, :], r=(), w=("xt%d" % sub,),
                         semkey="xt%d" % sub)
                self.norm_to_hT(xt[sub], "xt%d" % sub, V_MIXPRE, hT, "hT", sub, hs, "hs")
            items = [(OFF_U + g * 128, ubuf[:, g, 16:272], "ubuf%d" % g) for g in range(4)]
            items += [(OFF_Q + h * 128, qT[:, h, :], qTk) for h in range(8)]
            items += [(OFF_K, KT[:, t0:t0 + 256], "KT%d" % c)]
            items += [(OFF_QI + j * 128, qiT[:, j, :], "qiT") for j in range(2)]
            for off, dst, dk in items:
                b = 4 + nxt("J", 2)
                for k in range(8):
                    self.mm(ps[:, b, 0:256], Wa[:, k, off:off + 128], hT[:, k, :], k == 0, k == 7, r=("Wa", "hT"), w=("ps%d" % b,))
                self.add("act", lambda e, dst=dst, b=b: e.activation(dst, ps[:, b, 0:256], AF.Copy), r=("ps%d" % b,), w=(dk,))
            b = 4 + nxt("J", 2)
            for half in range(2):
                for k in range(8):
                    self.mm(ps[half * 64:(half + 1) * 64, b, 0:256], Wa[:, k, OFF_KI:OFF_KI + 64], hT[:, k, :], k == 0, k == 7,
                            r=("Wa", "hT"), w=("ps%d" % b,))
            self.add("act", lambda e, b=b, t0=t0: e.activation(kiT[:, t0:t0 + 256], ps[:, b, 0:256], AF.Copy), r=("ps%d" % b,), w=("kiT%d" % c,))
            for sub in range(2):
                b = 4 + nxt("J", 2)
                for k in range(8):
                    self.mm(ps[:, b, 0:128], hT[:, k, sub * 128:(sub + 1) * 128], Wa[:, k, OFF_V:OFF_V + 128], k == 0, k == 7,
                            r=("Wa", "hT"), w=("ps%d" % b,))
                for k in range(8):
                    self.mm(ps[:, b, 128:132], hT[:, k, sub * 128:(sub + 1) * 128], Wa[:, k, OFF_WI:OFF_WI + 4], k == 0, k == 7,
                            r=("Wa", "hT"), w=("ps%d" % b,))
                qi = 2 * c + sub
                self.add("act", lambda e, b=b, qi=qi: e.activation(V[:, qi, 0:128], ps[:, b, 0:128], AF.Copy), r=("ps%d" % b,), w=("V%d" % c,))
                self.add("act", lambda e, b=b, sub=sub: e.activation(widx[:, sub, :], ps[:, b, 128:132], AF.Copy), r=("ps%d" % b,), w=("widx",))
                idb = bass.AP(identb.tensor, 0, [[128, 128], [0, 4], [1, 128]])
                wb = bass.AP(widx.tensor, sub * 4, [[8, 128], [1, 4], [0, 128]])
                self.add("pool", lambda e, sub=sub, idb=idb, wb=wb: e.tensor_tensor(diagw[sub], idb, wb, ALU.mult),
                         r=("identb", "widx"), w=("diagw%d" % sub,))
            for g in range(4):
                uk = "ubuf%d" % g
                u = ubuf[:, g, :]
                bufs = [(pa, "pa"), (pb, "pb")]
                cur, curk, lo_ = u, uk, 0
                for lvl in range(g + 1):
                    sh = 1 << lvl
                    dstb, dstk = bufs[lvl % 2]
                    lo2 = lo_ + sh
                    self.add("pool", lambda e, dstb=dstb, cur=cur, lo2=lo2, sh=sh: e.tensor_tensor(
                        dstb[:, lo2:272], cur[:, lo2:272], cur[:, lo2 - sh:272 - sh], ALU.add), r=(curk,), w=(dstk,))
                    cur, curk, lo_ = dstb, dstk, lo2
                w = float(2 << g)
                self.add("pool", lambda e, cur=cur, w=w: e.tensor_scalar(
                    cur[:, 16:272], cur[:, 16:272], 1.0 / w, None, ALU.mult), r=(curk,), w=(curk,))
                self.add("pool", lambda e, cur=cur, u=u, g=g: e.tensor_tensor(
                    pooledT[:, g, :], cur[:, 16:272], u[:, 16:272], ALU.subtract), r=(curk, uk), w=("pooledT",))
                if c == 0:
                    self.add("pool", lambda e, cur=cur, g=g: e.tensor_tensor(
                        t16, cur[:, 16:32], cst[:, C_INVC + g * 16:C_INVC + (g + 1) * 16], ALU.mult), r=(curk, "consts"), w=("t16",))
                    self.add("pool", lambda e, w=w: e.tensor_scalar(t16, t16, w, None, ALU.mult), r=("t16",), w=("t16",))
                    self.add("pool", lambda e, u=u, g=g: e.tensor_tensor(pooledT[:, g, 0:16], t16, u[:, 16:32], ALU.subtract),
                             r=("t16", uk), w=("pooledT",))
                self.add("pool", lambda e, u=u: e.tensor_copy(u[:, 0:16], u[:, 256:272]), r=(uk,), w=(uk,))
            pm = pmT[c % 2]
            pmk = "pmT%d" % (c % 2)
            for g in range(4):
                b = 4 + nxt("J", 2)
                self.mm(ps[:, b, 0:256], Wpool[:, g, :], pooledT[:, g, :], True, True, r=("Wpool", "pooledT"), w=("ps%d" % b,))
                sc_ = self.vT[:, V_PSCALE * 8 + g:V_PSCALE * 8 + g + 1]
                self.add("act", lambda e, g=g, b=b, sc_=sc_, pm=pm: e.activation(pm[:, g, :], ps[:, b, 0:256], AF.Copy, scale=sc_),
                         r=("ps%d" % b, "vT"), w=(pmk,))
            self.dma(self.PMd[:, :, t0:t0 + 256].rearrange("g p t -> p g t"), pm, r=(pmk,), w=(), semkey=pmk)
            jobs = []
            for sub in range(2):
                qi = 2 * c + sub
                n = 128 * (qi + 1)
                nch = (n + 511) // 512
                for kc in range(nch):
                    jobs.append((sub, n, kc, min(512, n - kc * 512), kc == nch - 1))
            jst = {}

            def dots(j):
                sub, n, kc, cols, last = jobs[j]
                ri = nxt("R", 2)
                R_ = Rb[ri]
                X = (0, 1, 2, 3) if nxt("S", 2) == 0 else (4, 5, 6, 7)
                jst[j] = (R_, ri, X)
                for h in range(4):
                    p0 = (h % 2) * 64
                    self.mm(ps[:, X[h], 0:cols], qiT[p0:p0 + 64, h // 2, sub * 128:(sub + 1) * 128],
                            kiT[p0:p0 + 64, kc * 512:kc * 512 + cols], True, True,
                            r=("qiT",) + tuple("kiT%d" % jj for jj in range(2 * kc, min(2 * kc + 2, c + 1))), w=("ps%d" % X[h],))
                    self.add("act", lambda e, R_=R_, h=h, cols=cols, X=X: e.activation(R_[:, h, 0:cols], ps[:, X[h], 0:cols], AF.Relu),
                             r=("ps%d" % X[h],), w=("R%d_%d" % (ri, h),))

            def smm(j):
                sub, n, kc, cols, last = jobs[j]
                R_, ri, X = jst[j]
                bS = X[0]
                for h in range(4):
                    self.mm(ps[:, bS, 0:cols], diagw[sub][:, h, :], R_[:, h, 0:cols], h == 0, (h == 3 and not last),
                            r=("diagw%d" % sub, "R%d_%d" % (ri, h)), w=("ps%d" % bS,))
                if last:
                    dc = n - 128 - kc * 512
                    self.mm(ps[:, bS, dc:dc + 128], identb, causb, False, True, r=("identb", "causb"), w=("ps%d" % bS,))
                self.add("act", lambda e, sub=sub, kc=kc, cols=cols, bS=bS: e.activation(
                    score[sub][:, kc * 512:kc * 512 + cols], ps[:, bS, 0:cols], AF.Copy), r=("ps%d" % bS,), w=("score%d" % sub,))

            los = [None, None]
            st = [None, None]
            kd = [0, 0]
            BS[c] = [los, st, kd]
            last0 = max(j for j in range(len(jobs)) if jobs[j][0] == 0)
            dots(0)
            for j in range(len(jobs)):
                if j + 1 < len(jobs):
                    dots(j + 1)
                smm(j)
                if j == last0:
                    bsetup(c, 0)
                elif j > last0 and st[0] is not None and kd[0] < NB - 2:
                    F2one(c, 0)
            bsetup(c, 1)

        def F2fin(c):
            los, st, kd = BS[c]
            for sub in range(2):
                if st[sub] is None:
                    continue
                mid, midk, hk, hkk, n, sk = st[sub]
                lo, lok = self.stat_slot()
                self.add("dve", lambda e, lo=lo, mid=mid, hk=hk: e.tensor_tensor(lo, mid, hk[:, NB:NB + 1], ALU.subtract),
                         r=(midk, hkk), w=(lok,))
                los[sub] = (lo, lok)

        def F3(c):
            los, st, kd = BS[c]
            maskT = maskTs[c % 2]
            mTk = "maskT%d" % (c % 2)
            for sub in range(2):
                qi = 2 * c + sub
                n = 128 * (qi + 1)
                nch = (n + 511) // 512
                lo, lok = los[sub]
                for kc in range(nch):
                    cols = min(512, n - kc * 512)
                    mi = nxt("T", 2)
                    mc = maskc[mi]
                    mck = "maskc%d" % mi
                    self.add("dve", lambda e, mc=mc, sub=sub, kc=kc, cols=cols, lo=lo: e.tensor_scalar(
                        mc[:, 0:cols], score[sub][:, kc * 512:kc * 512 + cols], lo, None, ALU.is_ge), r=("score%d" % sub, lok), w=(mck,))
                    bT = 6 + mi
                    nb4 = cols // 128
                    for b4 in range(nb4):
                        self.tr(ps[:, bT, b4 * 128:(b4 + 1) * 128], mc[:, b4 * 128:(b4 + 1) * 128], r=(mck,), w=("ps%d" % bT,))
                    src = ps[:, bT, 0:cols].rearrange("p (k t) -> p k t", k=nb4)
                    self.add("act", lambda e, src=src, kc=kc, nb4=nb4, sub=sub: e.activation(
                        maskT[:, kc * 4:kc * 4 + nb4, sub * 128:(sub + 1) * 128], src, AF.Copy), r=("ps%d" % bT,), w=(mTk,))

        def AH(c, h):
            qT = qTs[c % 2]
            qTk = "qT%d" % (c % 2)
            maskT = maskTs[c % 2]
            mTk = "maskT%d" % (c % 2)
            nkt = 2 * c + 2
            LA = 3
            bO = (4, 5) if h % 2 == 0 else (6, 7)
            slots = {}

            def front(kt, h=h):
                q0 = 0 if kt <= 2 * c else 128
                wq = 256 - q0
                bL, cL = nxt("L", 4), 0
                lk = "ps%d" % bL
                sl = nxt("P", 4)
                slots[kt] = (sl, q0)
                self.mm(ps[:, bL, cL:cL + wq], KT[:, kt * 128:(kt + 1) * 128], qT[:, h, q0:256], True, True,
                        r=("KT%d" % (kt // 2), qTk), w=(lk,))
                self.add("act", lambda e: e.activation(PT[sl][:, 0:wq], ps[:, bL, cL:cL + wq], AF.Exp, scale=SC),
                         r=(lk,), w=("PT%d" % sl,))
                me = "pool"
                self.add(me, lambda e: e.tensor_tensor(PTm[sl][:, 0:wq], PT[sl][:, 0:wq], maskT[:, kt, q0:256], ALU.mult),
                         r=("PT%d" % sl, mTk), w=("PTm%d" % sl,))
                eb = None
                if kt == 2 * c:
                    eb = (0, 256, 0)
                elif kt == 2 * c + 1:
                    eb = (0, 128, 0)
                elif kt == 2 * c - 1:
                    eb = (0, 128, 128)
                if eb is not None:
                    a0, a1, e0 = eb
                    self.add(me, lambda e: e.tensor_tensor(PTm[sl][:, a0:a1], PTm[sl][:, a0:a1], EB[:, h, e0:e0 + (a1 - a0)], ALU.mult),
                             r=("PTm%d" % sl, "EB"), w=("PTm%d" % sl,))

            def back(kt, h=h, bO=bO):
                sl, q0 = slots[kt]
                for s_ in range(2):
                    if kt > 2 * c + s_:
                        continue
                    c0 = s_ * 128 - q0
                    self.mm(ps[:, bO[s_], 0:129], PTm[sl][:, c0:c0 + 128], V[:, kt, 0:129], kt == 0, kt == 2 * c + s_,
                            r=("PTm%d" % sl, "V%d" % (kt // 2)), w=("ps%d" % bO[s_],))

            for kt in range(min(LA, nkt)):
                front(kt)
            for kt in range(nkt):
                back(kt)
                if kt + LA < nkt:
                    front(kt + LA)

        def AHN(c, h):
            bO = (4, 5) if h % 2 == 0 else (6, 7)
            for s_ in range(2):
                rd, rdk = self.stat_slot()
                self.add("dve", lambda e, rd=rd, b=bO[s_]: e.reciprocal(rd, ps[:, b, 128:129]), r=("ps%d" % bO[s_],), w=(rdk,))
                self.add("dve", lambda e, rd=rd, b=bO[s_], s_=s_, h=h: e.tensor_scalar(
                    att[s_][:, h * 128:(h + 1) * 128], ps[:, b, 0:128], rd, None, ALU.mult), r=("ps%d" % bO[s_], rdk), w=("att%d" % s_,))

        def AFN(c):
            t0 = c * 256
            at = atT[c % 2]
            atk = "atT%d" % (c % 2)
            for s_ in range(2):
                for half in range(2):
                    b = 6 + half
                    for hh in range(4):
                        h = half * 4 + hh
                        self.tr(ps[:, b, hh * 128:(hh + 1) * 128], att[s_][:, h * 128:(h + 1) * 128], r=("att%d" % s_,), w=("ps%d" % b,))
                    src = ps[:, b, :].rearrange("p (h t) -> p h t", h=4)
                    self.add("act", lambda e, src=src, half=half, s_=s_, at=at: e.activation(
                        at[:, half * 4:half * 4 + 4, s_ * 128:(s_ + 1) * 128], src, AF.Copy), r=("ps%d" % b,), w=(atk,))
            self.dma(self.ATd[:, :, t0:t0 + 256].rearrange("g p t -> p g t"), at, r=(atk,), w=(), semkey=atk)


        NCH = S // 256
        F1(0)
        F2some(0, F2rem(0))
        F2fin(0)
        F3(0)
        for c in range(NCH):
            nx = c + 1 < NCH
            if nx:
                F1(c + 1)
                rem = F2rem(c + 1)
            for h in range(8):
                if nx:
                    F2some(c + 1, ((h + 1) * rem) // 8 - (h * rem) // 8)
                AH(c, h)
                if h > 0:
                    AHN(c, h - 1)
            AHN(c, 7)
            if nx:
                F2fin(c + 1)
                F3(c + 1)
            AFN(c)

    def pass_B(self):
        self.common_alloc()
        ps = self.ps
        Wg = self.sb("Wg", [128, 8, 2048], BF16)
        Wpp = self.sb("Wpp", [128, 4, D], BF16)
        Wpa = self.sb("Wpa", [128, 8, D], BF16)
        Wout = self.sb("Wout", [128, 8, D], BF16)
        Wqm = self.sb("Wqm", [128, 8, 512], BF16)
        Wom = self.sb("Wom", [128, 4, D], BF16)
        Wkv = self.sb("Wkv", [128, 8, D], BF16)
        self.load_w("Wkv", Wkv, self.w_kv_mem, 8)
        self.load_w("Wg", Wg, self.w_in[:, OFF_GP:OFF_GP + 2048], 8)
        self.load_w("Wpp", Wpp, self.w_proj_pool, 4)
        self.load_w("Wpa", Wpa, self.w_proj_attn, 8)
        self.load_w("Wout", Wout, self.w_out, 8)
        self.load_w("Wqm", Wqm, self.w_q_mem, 8)
        self.load_w("Wom", Wom, self.w_o_mem, 4)
        gbc1 = self.gbc_load("gbc1", V_MIXPOST)
        gbc2 = self.gbc_load("gbc2", V_MEMPOST)
        xt = [self.sb("xt%d" % i, [128, D], F32) for i in range(4)]
        hs = self.sb("hs", [128, D], F32)
        tmp = self.sb("tmp", [128, D], F32)
        hTa = [self.sb("hTa%d" % i, [128, 8, 256], BF16) for i in range(2)]
        hTb = self.sb("hTb", [128, 8, 256], BF16)
        hT = hTb
        hTk = "hTb"
        pmT = [self.sb("pmT%d" % i, [128, 4, 256], BF16) for i in range(2)]
        atT = [self.sb("atT%d" % i, [128, 8, 256], BF16) for i in range(2)]
        mgT = self.sb("mgT", [128, 8, 256], BF16)
        sg = [self.sb("sg%d" % i, [128, 512], F32) for i in range(2)]
        mm_ = [self.sb("mm%d" % i, [128, 512], F32) for i in range(2)]
        memKT = self.sb("memKT", [128, 4, NMEM], BF16)
        Vm = self.sb("Vm", [128, 2, 4, 130], BF16)
        qmT = self.sb("qmT", [128, 4, 256], BF16)
        PmT = [self.sb("PmT%d" % i, [128, 2, 256], BF16) for i in range(2)]
        o = [self.sb("o%d" % i, [128, 512], F32) for i in range(2)]
        oT = self.sb("oT", [128, 4, 256], BF16)
        SC = float(128 ** -0.5)

        self.add("pool", lambda e: e.memset(Vm, 1.0), w=("Vm",))
        for mt in range(2):
            self.dma(xt[mt][:, :], self.mem[mt * 128:(mt + 1) * 128, :], r=(), w=("xt%d" % mt,), semkey="xt%d" % mt)
            self.norm_to_hT(xt[mt], "xt%d" % mt, V_MEMKV, hT, hTk, mt, hs, "hs")
        for h in range(4):
            b = h % 2
            for k in range(8):
                self.mm(ps[:, b, 0:256], Wkv[:, k, h * 128:(h + 1) * 128], hT[:, k, :], k == 0, k == 7,
                        r=("Wkv", hTk), w=("ps%d" % b,))
            self.add("act", lambda e, h=h, b=b: e.activation(memKT[:, h, :], ps[:, b, 0:256], AF.Copy),
                     r=("ps%d" % b,), w=("memKT",))
        for mt in range(2):
            b = 2 + mt
            for k in range(8):
                self.mm(ps[:, b, :], hT[:, k, mt * 128:(mt + 1) * 128], Wkv[:, k, 512:1024], k == 0, k == 7,
                        r=("Wkv", hTk), w=("ps%d" % b,))
            src = ps[:, b, :].rearrange("p (h d) -> p h d", h=4)
            self.add("act", lambda e, mt=mt, src=src: e.activation(Vm[:, mt, :, 0:128], src, AF.Copy),
                     r=("ps%d" % b,), w=("Vm",))

        def load_norm(c):
            t0 = c * 256
            pm = pmT[c % 2]
            pmk = "pmT%d" % (c % 2)
            at = atT[c % 2]
            atk = "atT%d" % (c % 2)
            hT = hTa[c % 2]
            hTk = "hTa%d" % (c % 2)
            xs = [(xt[(c % 2) * 2 + sub], "xt%d" % ((c % 2) * 2 + sub)) for sub in range(2)]
            self.dma(pm, self.PMd[:, :, t0:t0 + 256].rearrange("g p t -> p g t"), r=(), w=(pmk,), semkey=pmk)
            self.dma(at, self.ATd[:, :, t0:t0 + 256].rearrange("g p t -> p g t"), r=(), w=(atk,), semkey=atk)
            for sub in range(2):
                self.dma(xs[sub][0][:, :], self.x[t0 + sub * 128:t0 + (sub + 1) * 128, :], r=(), w=(xs[sub][1],), semkey=xs[sub][1])
                self.norm_to_hT(xs[sub][0], xs[sub][1], V_MIXPRE, hT, hTk, sub, hs, "hs", banks=(2, 3))

        def merge_out(c):
            t0 = c * 256
            pm = pmT[c % 2]
            pmk = "pmT%d" % (c % 2)
            at = atT[c % 2]
            atk = "atT%d" % (c % 2)
            hT = hTa[c % 2]
            hTk = "hTa%d" % (c % 2)
            xs = [(xt[(c % 2) * 2 + sub], "xt%d" % ((c % 2) * 2 + sub)) for sub in range(2)]
            for j in range(8):
                bA = (2 * j) % 4
                bB = bA + 1
                for k in range(8):
                    self.mm(ps[:, bA, 0:256], Wg[:, k, j * 128:(j + 1) * 128], hT[:, k, :], k == 0, k == 7,
                            r=("Wg", hTk), w=("ps%d" % bA,))
                for k in range(8):
                    self.mm(ps[:, bA, 256:512], Wg[:, k, 1024 + j * 128:1024 + (j + 1) * 128], hT[:, k, :], k == 0, k == 7,
                            r=("Wg", hTk), w=("ps%d" % bA,))
                for k in range(4):
                    self.mm(ps[:, bB, 0:256], Wpp[:, k, j * 128:(j + 1) * 128], pm[:, k, :], k == 0, k == 3,
                            r=("Wpp", pmk), w=("ps%d" % bB,))
                for k in range(8):
                    self.mm(ps[:, bB, 256:512], Wpa[:, k, j * 128:(j + 1) * 128], at[:, k, :], k == 0, k == 7,
                            r=("Wpa", atk), w=("ps%d" % bB,))
                s_ = sg[j % 2]
                sk = "sg%d" % (j % 2)
                m_ = mm_[j % 2]
                mk = "mm%d" % (j % 2)
                self.add("act", lambda e, s_=s_, bA=bA: e.activation(s_, ps[:, bA, :], AF.Sigmoid), r=("ps%d" % bA,), w=(sk,))
                self.add("dve", lambda e, s_=s_, m_=m_, bB=bB: e.tensor_tensor(m_, s_, ps[:, bB, :], ALU.mult),
                         r=(sk, "ps%d" % bB), w=(mk,))
                self.add("pool", lambda e, m_=m_, j=j: e.tensor_tensor(mgT[:, j, :], m_[:, 0:256], m_[:, 256:512], ALU.add),
                         r=(mk,), w=("mgT",))
            for sub in range(2):
                yb = (4, 5) if sub == 0 else (6, 7)
                for half in range(2):
                    for k in range(8):
                        self.mm(ps[:, yb[half], :], mgT[:, k, sub * 128:(sub + 1) * 128], Wout[:, k, half * 512:(half + 1) * 512],
                                k == 0, k == 7, r=("mgT", "Wout"), w=("ps%d" % yb[half],))
                self.post_norm_residual(yb, gbc1, "gbc1", xs[sub][0], xs[sub][1], tmp, "tmp")

        def memattn(c):
            t0 = c * 256
            pm = pmT[c % 2]
            pmk = "pmT%d" % (c % 2)
            at = atT[c % 2]
            atk = "atT%d" % (c % 2)
            hT = hTa[c % 2]
            hTk = "hTa%d" % (c % 2)
            xs = [(xt[(c % 2) * 2 + sub], "xt%d" % ((c % 2) * 2 + sub)) for sub in range(2)]
            hT = hTb
            hTk = "hTb"
            for sub in range(2):
                self.norm_to_hT(xs[sub][0], xs[sub][1], V_MEMPRE, hT, hTk, sub, hs, "hs", banks=(0, 1))
            for h in range(4):
                b = 2 + h % 2
                for k in range(8):
                    self.mm(ps[:, b, 0:256], Wqm[:, k, h * 128:(h + 1) * 128], hT[:, k, :], k == 0, k == 7,
                            r=("Wqm", hTk), w=("ps%d" % b,))
                self.add("act", lambda e, h=h, b=b: e.activation(qmT[:, h, :], ps[:, b, 0:256], AF.Copy),
                         r=("ps%d" % b,), w=("qmT",))
            for h in range(4):
                bL = h % 2
                P_ = PmT[h % 2]
                Pk = "PmT%d" % (h % 2)
                for mt in range(2):
                    self.mm(ps[:, bL, mt * 256:(mt + 1) * 256], memKT[:, h, mt * 128:(mt + 1) * 128], qmT[:, h, :], True, True,
                            r=("memKT", "qmT"), w=("ps%d" % bL,))
                src = ps[:, bL, :].rearrange("p (m t) -> p m t", m=2)
                self.add("act", lambda e, P_=P_, src=src: e.activation(P_, src, AF.Exp, scale=SC), r=("ps%d" % bL,), w=(Pk,))
                for sub in range(2):
                    bO = 4 + sub * 2 + (h // 3)
                    col = (h % 3) * 130
                    for mt in range(2):
                        self.mm(ps[:, bO, col:col + 129], P_[:, mt, sub * 128:(sub + 1) * 128], Vm[:, mt, h, 0:129],
                                mt == 0, mt == 1, r=(Pk, "Vm"), w=("ps%d" % bO,))
                    rd, rdk = self.stat_slot()
                    self.add("dve", lambda e, rd=rd, bO=bO, col=col: e.reciprocal(rd, ps[:, bO, col + 128:col + 129]),
                             r=("ps%d" % bO,), w=(rdk,))
                    self.add("dve", lambda e, rd=rd, bO=bO, col=col, sub=sub, h=h: e.tensor_scalar(
                        o[sub][:, h * 128:(h + 1) * 128], ps[:, bO, col:col + 128], rd, None, ALU.mult),
                        r=("ps%d" % bO, rdk), w=("o%d" % sub,))
            for sub in range(2):
                b = sub
                for h in range(4):
                    self.tr(ps[:, b, h * 128:(h + 1) * 128], o[sub][:, h * 128:(h + 1) * 128], r=("o%d" % sub,), w=("ps%d" % b,))
                src = ps[:, b, :].rearrange("p (h t) -> p h t", h=4)
                self.add("act", lambda e, src=src, sub=sub: e.activation(oT[:, :, sub * 128:(sub + 1) * 128], src, AF.Copy),
                         r=("ps%d" % b,), w=("oT",))
            for sub in range(2):
                yb = (4, 5) if sub == 0 else (6, 7)
                for half in range(2):
                    for k in range(4):
                        self.mm(ps[:, yb[half], :], oT[:, k, sub * 128:(sub + 1) * 128], Wom[:, k, half * 512:(half + 1) * 512],
                                k == 0, k == 3, r=("oT", "Wom"), w=("ps%d" % yb[half],))
                self.post_norm_residual(yb, gbc2, "gbc2", xs[sub][0], xs[sub][1], tmp, "tmp")
                dst = self.X2d if "C" in self.stages or True else self.out
                self.dma(dst[t0 + sub * 128:t0 + (sub + 1) * 128, :], xs[sub][0][:, :], r=(xs[sub][1],), w=(),
                         semkey=xs[sub][1])


        NCH = S // 256
        load_norm(0)
        for c in range(NCH):
            merge_out(c)
            if c + 1 < NCH:
                load_norm(c + 1)
            memattn(c)
    def build(self):
        self.bodies = []
        if "A" in self.stages:
            self.bodies.append(self.pass_A)
        if "B" in self.stages:
            self.bodies.append(self.pass_B)
        if "C" in self.stages:
            self.bodies.append(self.pass_C)
        for b in self.bodies:
            self.run_pass(b)
        return self.nc


def make_in_maps(inputs):
    f = lambda a: np.ascontiguousarray(np.asarray(a, dtype=np.float32))
    vec_names = ["norm_mix_pre", "norm_mix_post", "norm_mem_pre", "norm_mem_kv", "norm_mem_post",
                 "norm_ffn_pre", "norm_ffn_post"]
    vecs = np.zeros((8, D), np.float32)
    for i, n in enumerate(vec_names):
        vecs[i] = f(inputs[n])
    vecs[V_PSCALE, :512] = f(inputs["pool_scale"])
    vecsT = np.ascontiguousarray(vecs.reshape(8, 8, 128).transpose(2, 0, 1).reshape(128, 64))
    shared = {k: f(inputs[k]) for k in ["w_in", "pool_w", "w_proj_pool", "w_proj_attn", "rel_bias_table", "w_out",
                                        "w_q_mem", "w_kv_mem", "w_o_mem", "w_gate_up", "w_down"]}
    shared["vecs"] = vecs
    shared["vecsT"] = vecsT
    shared["consts"] = make_consts()
    x = f(inputs["x"])
    mem = f(inputs["mem"])
    maps = []
    for b in range(x.shape[0]):
        m = dict(shared)
        m["x"] = x[b]
        m["mem"] = mem[b]
        maps.append(m)
    return maps


_NC_CACHE = {}


def kernel(**inputs):
    if "full" not in _NC_CACHE:
        _NC_CACHE["full"] = Builder("ABC").build()
    nc = _NC_CACHE["full"]
    maps = make_in_maps(inputs)
    res = run_bass_kernel_spmd(nc, maps, core_ids=list(range(len(maps))))
    return np.stack([np.asarray(r["out"], dtype=np.float32) for r in res.results], axis=0)
```

```python
import numpy as np
import concourse.bass as bass
import concourse.mybir as mybir
from concourse.bass_utils import run_bass_kernel_spmd

F32 = mybir.dt.float32
BF16 = mybir.dt.bfloat16
ALU = mybir.AluOpType
AF = mybir.ActivationFunctionType
AX = mybir.AxisListType

S = 4096
D = 1024
NT = S // 128
DFF = 2816
NF = DFF // 128
NMEM = 256
EPS = 1e-6
IN_W = 4164
OFF_U, OFF_Q, OFF_K, OFF_V, OFF_QI, OFF_KI, OFF_WI, OFF_GP, OFF_GA = 0, 512, 1536, 1664, 1792, 2048, 2112, 2116, 3140
TOPK = 256
NBISECT = 18
BIG = 30000.0

ENGS = ("pe", "act", "dve", "pool", "sp")
SEM_EPOCH = 30000


class Op:
    __slots__ = ("eng", "fn", "dma", "semkey", "deps", "sig", "needed", "idx")

    def __init__(self, eng, fn, dma, semkey):
        self.eng = eng
        self.fn = fn
        self.dma = dma
        self.semkey = semkey
        self.deps = {}
        self.sig = None
        self.needed = False


class Prog:
    def __init__(self, nc):
        self.nc = nc
        self.ops = []
        self.lastw = {}
        self.readers = {}
        self.sems = {}
        self.pass_start = 0
        self.cnt = {e: 0 for e in ENGS}
        self.dcnt = {}
        self.waited = {e: {} for e in ENGS}

    def add(self, eng, fn, r=(), w=(), dma=False, semkey=None):
        op = Op(eng, fn, dma, semkey)
        op.idx = len(self.ops)
        for k in r:
            lw = self.lastw.get(k)
            if lw is not None:
                op.deps[lw.idx] = (lw, "RAW")
        for k in w:
            lw = self.lastw.get(k)
            if lw is not None:
                op.deps[lw.idx] = (lw, "WAW")
            for rd in self.readers.get(k, ()):
                if rd.idx not in op.deps:
                    op.deps[rd.idx] = (rd, "WAR")
        for k in r:
            self.readers.setdefault(k, []).append(op)
        for k in w:
            self.lastw[k] = op
            self.readers[k] = []
        self.ops.append(op)
        return op

    def _sem(self, name):
        s = self.sems.get(name)
        if s is None:
            s = self.nc.alloc_semaphore(name)
            self.sems[name] = s
        return s

    def barrier(self):
        last = {}
        for op in self.ops[self.pass_start:]:
            last[op.eng] = op
            if op.dma:
                last["dma_" + op.semkey] = op
        fences = list(last.values())
        for e in ENGS:
            op = Op(e, None, False, None)
            op.idx = len(self.ops)
            for f in fences:
                op.deps[f.idx] = (f, "RAW")
            self.ops.append(op)
        self.lastw = {}
        self.readers = {}

    def flush(self, engines):
        ops = self.ops[self.pass_start:]
        self.pass_start = len(self.ops)
        for op in ops:
            keep = {}
            for i, (d, kind) in op.deps.items():
                if d is op:
                    continue
                if not d.dma and d.eng == op.eng:
                    if op.eng == "pe" and not op.dma:
                        continue
                keep[i] = (d, kind)
            op.deps = keep
            for d, _ in keep.values():
                d.needed = True
        for op in ops:
            if op.dma:
                c = self.dcnt.get(op.semkey, 0) + 1
                self.dcnt[op.semkey] = c
                op.sig = ("dma_" + op.semkey, 16 * c)
            elif op.needed:
                c = self.cnt[op.eng]
                self.cnt[op.eng] = c + 1
                op.sig = ("e_%s_%d" % (op.eng, c // SEM_EPOCH), c % SEM_EPOCH + 1)
        by = {e: [] for e in ENGS}
        for op in ops:
            by[op.eng].append(op)

        def replay(e, eo):
            waited = self.waited[e]
            for op in by[e]:
                need = {}
                for d, _ in op.deps.values():
                    sn, v = d.sig
                    if need.get(sn, 0) < v:
                        need[sn] = v
                for sn, v in need.items():
                    if waited.get(sn, 0) >= v:
                        continue
                    waited[sn] = v
                    eo.wait_ge(self._sem(sn), v)
                if op.fn is None:
                    continue
                ins = op.fn(eo)
                if op.sig is not None:
                    sn, v = op.sig
                    ins.then_inc(self._sem(sn), 16 if op.dma else 1)
        return replay

    def final_waits(self, eo):
        for k, c in self.dcnt.items():
            eo.wait_ge(self._sem("dma_" + k), 16 * c)


def sb_bcast_free(t, col0, ncol, rep):
    W = t.shape[1]
    return bass.AP(t, col0, [[W, 128], [1, ncol], [0, rep]])


C_IDENT, C_CAUS, C_CMAT, C_OHR, C_POW2, C_INVC = 0, 128, 256, 288, 672, 704
V_MIXPRE, V_MIXPOST, V_MEMPRE, V_MEMKV, V_MEMPOST, V_FFNPRE, V_FFNPOST, V_PSCALE = range(8)


def make_consts():
    c = np.zeros((128, 1024), np.float32)
    c[:, C_IDENT:C_IDENT + 128] = np.eye(128, dtype=np.float32)
    t = np.arange(128)[:, None]
    s = np.arange(128)[None, :]
    c[:, C_CAUS:C_CAUS + 128] = np.where(s <= t, 0.0, -BIG)
    cm = np.eye(32, dtype=np.float32)
    cm[31, :] -= 1.0
    c[0:32, C_CMAT:C_CMAT + 32] = cm
    d = np.maximum(255 - np.arange(383), 0)
    nf = np.maximum(d, 1).astype(np.float32)
    large = 16 + (np.log(nf / np.float32(16)) / np.float32(np.log(128 / 16)) * np.float32(16)).astype(np.int32)
    large = np.minimum(large, 31)
    bucket = np.where(d < 16, d, large)
    bucket = np.where(np.arange(383) > 255, 31, bucket)
    oh = np.zeros((32, 383), np.float32)
    oh[bucket, np.arange(383)] = 1.0
    c[0:32, C_OHR:C_OHR + 383] = oh
    c[:, C_POW2:C_POW2 + 32] = (0.5 ** (np.arange(32) + 1)).astype(np.float32)[None, :]
    for g, w in enumerate((2, 4, 8, 16)):
        c[:, C_INVC + g * 16:C_INVC + (g + 1) * 16] = (1.0 / np.minimum(np.arange(16) + 1, w)).astype(np.float32)[None, :]
    return c


class Builder:
    def __init__(self, stages="ABC", debug=False):
        self.stages = stages
        nc = bass.Bass("TRN2", target_bir_lowering=False)
        self.nc = nc
        self.P = Prog(nc)
        dt = nc.dram_tensor
        ei = "ExternalInput"
        self.x = dt("x", [S, D], F32, kind=ei).ap()
        self.mem = dt("mem", [NMEM, D], F32, kind=ei).ap()
        self.w_in = dt("w_in", [D, IN_W], F32, kind=ei).ap()
        self.pool_w = dt("pool_w", [4, 128, 128], F32, kind=ei).ap()
        self.w_proj_pool = dt("w_proj_pool", [512, D], F32, kind=ei).ap()
        self.w_proj_attn = dt("w_proj_attn", [D, D], F32, kind=ei).ap()
        self.rel_bias = dt("rel_bias_table", [32, 8], F32, kind=ei).ap()
        self.w_out = dt("w_out", [D, D], F32, kind=ei).ap()
        self.w_q_mem = dt("w_q_mem", [D, 512], F32, kind=ei).ap()
        self.w_kv_mem = dt("w_kv_mem", [D, D], F32, kind=ei).ap()
        self.w_o_mem = dt("w_o_mem", [512, D], F32, kind=ei).ap()
        self.w_gate_up = dt("w_gate_up", [D, 2 * DFF], F32, kind=ei).ap()
        self.w_down = dt("w_down", [DFF, D], F32, kind=ei).ap()
        self.vecs_t = dt("vecs", [8, D], F32, kind=ei)
        self.vecs = self.vecs_t.ap()
        self.vecsT = dt("vecsT", [128, 64], F32, kind=ei).ap()
        self.consts = dt("consts", [128, 1024], F32, kind=ei).ap()
        self.out = dt("out", [S, D], F32, kind="ExternalOutput").ap()

        def scratch(name, shape, dtype, producer):
            if not debug:
                kind = "Internal"
            else:
                kind = "ExternalOutput" if producer in stages else ei
            return dt(name, shape, dtype, kind=kind).ap()
        self.PMd = scratch("PMd", [4, 128, S], BF16, "A")
        self.ATd = scratch("ATd", [8, 128, S], BF16, "A")
        self.X2d = scratch("X2d", [S, D], F32, "B")

    def add(self, *a, **k):
        return self.P.add(*a, **k)

    def dma(self, out, in_, r, w, semkey, eng="sp", **kw):
        return self.P.add(eng, lambda e: e.dma_start(out=out, in_=in_, **kw), r=r, w=w, dma=True, semkey=semkey)

    def mm(self, out, lhsT, rhs, start, stop, r, w, **kw):
        return self.P.add("pe", lambda e: e.matmul(out, lhsT, rhs, start=start, stop=stop, **kw), r=r, w=w)

    def tr(self, out, in_, r, w):
        ident = self.ident
        return self.P.add("pe", lambda e: e.transpose(out, in_, ident), r=tuple(r) + ("consts",), w=w)

    def load_w(self, name, dst, src, nchunk):
        for c in range(nchunk):
            self.dma(dst[:, c, :], src[c * 128:(c + 1) * 128, :], r=(), w=(name,), semkey=name, eng="pool",
                     max_dma_last_dim=4096)

    def run_pass(self, body):
        from contextlib import ExitStack
        nc = self.nc
        with ExitStack() as st:
            self.st = st
            self.pname = body.__name__
            body()
            self.P.barrier()
            last = (body == self.bodies[-1])
            with nc.Block() as block:
                replay = self.P.flush(None)

                @block.tensor
                def _(e):
                    replay("pe", e)

                @block.scalar
                def _(e):
                    replay("act", e)

                @block.vector
                def _(e):
                    replay("dve", e)

                @block.gpsimd
                def _(e):
                    replay("pool", e)

                @block.sync
                def _(e):
                    replay("sp", e)
                    if last:
                        self.P.final_waits(e)

    def sb(self, name, shape, dtype):
        return self.st.enter_context(self.nc.sbuf_tensor("%s_%s" % (self.pname, name), list(shape), dtype)).ap()

    def common_alloc(self):
        nc = self.nc
        self.cst = self.sb("cst", [128, 1024], F32)
        self.ident = self.cst[:, C_IDENT:C_IDENT + 128]
        self.vT = self.sb("vT", [128, 64], F32)
        self.ps = self.st.enter_context(nc.psum_tensor("ps_" + self.pname, [128, 8, 512], F32)).ap()
        self.stat = self.sb("stat", [128, 256], F32)
        self.stat_i = 0
        self.junk = self.sb("junk", [128, 1024], F32)
        if self.pname != "pass_A":
            self.ysb = self.sb("ysb", [128, 1024], F32)
        self.dma(self.cst[:, :], self.consts[:, :], r=(), w=("consts",), semkey="consts")
        self.dma(self.vT[:, :], self.vecsT[:, :], r=(), w=("vT",), semkey="vT")

    def stat_slot(self, n=1):
        i = self.stat_i
        if i + n > 256:
            i = 0
        self.stat_i = i + n
        return self.stat[:, i:i + n], "stat%d" % i

    def gbc_load(self, name, row):
        t = self.sb(name, [128, D], F32)
        src = bass.AP(self.vecs_t, row * D, [[0, 128], [1, D]])
        self.dma(t[:, :], src, r=(), w=(name,), semkey=name)
        return t

    def rstd_of(self, ss, ssk, n):
        ms, msk = self.stat_slot()
        self.add("dve", lambda e: e.tensor_scalar(ms, ss, 1.0 / n, EPS, ALU.mult, ALU.add), r=(ssk,), w=(msk,))
        sd, sdk = self.stat_slot()
        self.add("act", lambda e: e.activation(sd, ms, AF.Sqrt), r=(msk,), w=(sdk,))
        rs, rsk = self.stat_slot()
        self.add("dve", lambda e: e.reciprocal(rs, sd), r=(sdk,), w=(rsk,))
        return rs, rsk

    def norm_to_hT(self, xt, xtk, vrow, hT, hTk, sub, hs, hsk, banks=(6, 7)):
        ss, ssk = self.stat_slot()
        junk = self.junk
        self.add("act", lambda e: e.activation(junk, xt, AF.Square, accum_out=ss), r=(xtk,), w=("junk", ssk))
        rs, rsk = self.rstd_of(ss, ssk, D)
        self.add("dve", lambda e: e.tensor_scalar(hs, xt, rs, None, ALU.mult), r=(xtk, rsk), w=(hsk,))
        for half in range(2):
            b = banks[half]
            pk = "ps%d" % b
            for jj in range(4):
                j = half * 4 + jj
                self.tr(self.ps[:, b, jj * 128:(jj + 1) * 128], hs[:, j * 128:(j + 1) * 128], r=(hsk,), w=(pk,))
            src = self.ps[:, b, :].rearrange("p (j t) -> p j t", j=4)
            g = bass.AP(self.vT.tensor, vrow * 8 + half * 4, [[64, 128], [1, 4], [0, 128]])
            dst = hT[:, half * 4:half * 4 + 4, sub * 128:(sub + 1) * 128]
            self.add("dve", lambda e, dst=dst, src=src, g=g: e.tensor_tensor(dst, src, g, ALU.mult),
                     r=(pk, "vT"), w=(hTk,))

    def post_norm_residual(self, ybanks, gbc, gbck, xt, xtk, tmp, tmpk):
        ysb = self.ysb
        for hf in range(2):
            b = ybanks[hf]
            y = self.ps[:, b, :]
            self.add("act", lambda e, y=y, hf=hf: e.activation(ysb[:, hf * 512:(hf + 1) * 512], y, AF.Copy),
                     r=("ps%d" % b,), w=("ysb",))
        ss, ssk = self.stat_slot()
        junk = self.junk
        self.add("act", lambda e: e.activation(junk, ysb, AF.Square, accum_out=ss), r=("ysb",), w=("junk", ssk))
        self.add("dve", lambda e: e.tensor_tensor(tmp, ysb, gbc, ALU.mult), r=("ysb", gbck), w=(tmpk,))
        rs, rsk = self.rstd_of(ss, ssk, D)
        self.add("dve", lambda e: e.scalar_tensor_tensor(xt, tmp, rs, xt, ALU.mult, ALU.add),
                 r=(tmpk, rsk, xtk), w=(xtk,))

    def pass_C(self):
        self.common_alloc()
        src = self.X2d if "B" in self.stages else self.x
        Wgu = self.sb("Wgu", [128, 8, 2 * DFF], BF16)
        Wd = self.sb("Wd", [128, NF, D], BF16)
        self.load_w("Wgu", Wgu, self.w_gate_up, 8)
        self.load_w("Wd", Wd, self.w_down, NF)
        gbc = self.gbc_load("gbcF", V_FFNPOST)
        xt = [self.sb("xt%d" % i, [128, D], F32) for i in range(4)]
        hs = self.sb("hs", [128, D], F32)
        tmp = self.sb("tmp", [128, D], F32)
        hT = [self.sb("hT%d" % i, [128, 8, 256], BF16) for i in range(2)]
        aT = self.sb("aT", [128, NF, 256], BF16)
        sg = [self.sb("sg%d" % i, [128, 256], F32) for i in range(3)]
        ps = self.ps
        def load_norm(c):
            t0 = c * 256
            for sub in range(2):
                xi = (c % 2) * 2 + sub
                self.dma(xt[xi][:, :], src[t0 + sub * 128:t0 + (sub + 1) * 128, :], r=(), w=("xt%d" % xi,),
                         semkey="xt%d" % xi)
                self.norm_to_hT(xt[xi], "xt%d" % xi, V_FFNPRE, hT[c % 2], "hT%d" % (c % 2), sub, hs, "hs", banks=(3, 3))

        def ffn1(c):
            h = hT[c % 2]
            hk = "hT%d" % (c % 2)
            for j in range(NF):
                b = j % 3
                pk = "ps%d" % b
                for k in range(8):
                    self.mm(ps[:, b, 0:256], Wgu[:, k, j * 128:(j + 1) * 128], h[:, k, :], k == 0, k == 7,
                            r=("Wgu", hk), w=(pk,))
                for k in range(8):
                    self.mm(ps[:, b, 256:512], Wgu[:, k, DFF + j * 128:DFF + (j + 1) * 128], h[:, k, :], k == 0, k == 7,
                            r=("Wgu", hk), w=(pk,))
                s_ = sg[j % 3]
                sk = "sg%d" % (j % 3)
                self.add("act", lambda e, s_=s_, b=b: e.activation(s_, ps[:, b, 0:256], AF.Silu), r=(pk,), w=(sk,))
                self.add("dve", lambda e, s_=s_, b=b, j=j: e.tensor_tensor(aT[:, j, :], s_, ps[:, b, 256:512], ALU.mult),
                         r=(pk, sk), w=("aT",))

        def ffn2(c):
            t0 = c * 256
            for sub in range(2):
                xi = (c % 2) * 2 + sub
                yb = (4, 5) if sub == 0 else (6, 7)
                for half in range(2):
                    pk = "ps%d" % yb[half]
                    for j in range(NF):
                        self.mm(ps[:, yb[half], :], aT[:, j, sub * 128:(sub + 1) * 128], Wd[:, j, half * 512:(half + 1) * 512],
                                j == 0, j == NF - 1, r=("aT", "Wd"), w=(pk,))
                self.post_norm_residual(yb, gbc, "gbcF", xt[xi], "xt%d" % xi, tmp, "tmp")
                self.dma(self.out[t0 + sub * 128:t0 + (sub + 1) * 128, :], xt[xi][:, :], r=("xt%d" % xi,), w=(),
                         semkey="xt%d" % xi)

        NCH = S // 256
        load_norm(0)
        for c in range(NCH):
            ffn1(c)
            if c + 1 < NCH:
                load_norm(c + 1)
            ffn2(c)

    def pass_A(self):
        self.common_alloc()
        ps = self.ps
        cst = self.cst
        NB = NBISECT
        SC = float(128 ** -0.5)
        WA = OFF_GP
        Wa = self.sb("Wa", [128, 8, WA], BF16)
        self.load_w("Wa", Wa, self.w_in[:, 0:WA], 8)
        Wpool = self.sb("Wpool", [128, 4, 128], BF16)
        for g in range(4):
            self.dma(Wpool[:, g, :], self.pool_w[g], r=(), w=("Wpool",), semkey="Wpool", eng="pool")
        KT = self.sb("KT", [128, S], BF16)
        V = self.sb("V", [128, NT, 130], BF16)
        kiT = self.sb("kiT", [128, S], BF16)
        xt = [self.sb("xt%d" % i, [128, D], F32) for i in range(2)]
        hs = self.sb("hs", [128, D], F32)
        hT = self.sb("hT", [128, 8, 256], BF16)
        ubuf = self.sb("ubuf", [128, 4, 272], F32)
        pa = self.sb("pa", [128, 272], F32)
        pb = self.sb("pb", [128, 272], F32)
        t16 = self.sb("t16", [128, 16], F32)
        pooledT = self.sb("pooledT", [128, 4, 256], BF16)
        pmT = [self.sb("pmT%d" % i, [128, 4, 256], BF16) for i in range(2)]
        qTs = [self.sb("qT%d" % i, [128, 8, 256], BF16) for i in range(2)]
        qiT = self.sb("qiT", [128, 2, 256], BF16)
        widx = self.sb("widx", [128, 2, 4], F32)
        diagw = [self.sb("diagw%d" % i, [128, 4, 128], BF16) for i in range(2)]
        identb = self.sb("identb", [128, 128], BF16)
        causb = self.sb("causb", [128, 128], BF16)
        score = [self.sb("score%d" % i, [128, S], F32) for i in range(2)]
        Rb = [self.sb("R%d" % i, [128, 4, 512], BF16) for i in range(2)]
        maskc = [self.sb("maskc%d" % i, [128, 512], F32) for i in range(2)]
        maskTs = [self.sb("maskT%d" % i, [128, NT, 256], BF16) for i in range(2)]
        PT = [self.sb("PT%d" % i, [128, 256], BF16) for i in range(4)]
        PTm = [self.sb("PTm%d" % i, [128, 256], BF16) for i in range(4)]
        att = [self.sb("att%d" % i, [128, D], F32) for i in range(2)]
        atT = [self.sb("atT%d" % i, [128, 8, 256], BF16) for i in range(2)]
        EB = self.sb("EB", [128, 8, 256], BF16)
        tbl = self.sb("tbl", [128, 8], F32)
        tblD = self.sb("tblD", [128, 8], F32)
        HK = [self.sb("HK%d" % i, [128, 32], F32) for i in range(2)]

        self.add("pool", lambda e: e.memset(V, 1.0), w=tuple("V%d" % i for i in range(S // 256)))
        self.add("pool", lambda e: e.memset(ubuf, 0.0), w=tuple("ubuf%d" % g for g in range(4)))
        self.add("dve", lambda e: e.tensor_copy(identb, cst[:, C_IDENT:C_IDENT + 128]), r=("consts",), w=("identb",))
        self.add("dve", lambda e: e.tensor_copy(causb, cst[:, C_CAUS:C_CAUS + 128]), r=("consts",), w=("causb",))
        self.dma(tbl[0:32, :], self.rel_bias[:, :], r=(), w=("tbl",), semkey="tbl")
        self.mm(ps[0:32, 0, 0:8], cst[0:32, C_CMAT:C_CMAT + 32], tbl[0:32, :], True, True, r=("consts", "tbl"), w=("ps0",))
        self.add("act", lambda e: e.activation(tblD[0:32, :], ps[0:32, 0, 0:8], AF.Copy), r=("ps0",), w=("tblD",))
        for jj in range(256):
            b = jj // 64
            self.mm(ps[:, b, (jj % 64) * 8:(jj % 64) * 8 + 8], cst[0:32, C_OHR + 255 - jj:C_OHR + 383 - jj], tblD[0:32, :],
                    True, True, r=("consts", "tblD"), w=("ps%d" % b,))
        for b in range(4):
            src = ps[:, b, :].rearrange("p (j h) -> p h j", h=8)
            self.add("act", lambda e, b=b, src=src: e.activation(EB[:, :, b * 64:(b + 1) * 64], src, AF.Exp),
                     r=("ps%d" % b,), w=("EB",))

        def BK(b):
            return ("ps%d" % b,)

        rot = {"L": 0, "P": 0, "T": 0, "J": 0, "R": 0, "S": 0, "M": 0}

        def nxt(k, n):
            v = rot[k]
            rot[k] = (v + 1) % n
            return v

        BS = {}

        def bsetup(c, sub):
            los, st, kd = BS[c]
            qi = 2 * c + sub
            n = 128 * (qi + 1)
            sk = "score%d" % sub
            if qi < 2:
                lo, lok = self.stat_slot()
                self.add("dve", lambda e, lo=lo: e.memset(lo, -10000.0), w=(lok,))
                los[sub] = (lo, lok)
                return
            mx, mxk = self.stat_slot()
            mn, mnk = self.stat_slot()
            self.add("dve", lambda e: e.tensor_reduce(mx, score[sub][:, 0:n], AX.X, ALU.max), r=(sk,), w=(mxk,))
            self.add("dve", lambda e: e.tensor_reduce(mn, score[sub][:, 0:n - 128], AX.X, ALU.min), r=(sk,), w=(mnk,))
            w0, w0k = self.stat_slot()
            self.add("dve", lambda e: e.scalar_tensor_tensor(w0, mx, 1.0, mn, ALU.add, ALU.subtract), r=(mxk, mnk), w=(w0k,))
            hk = HK[sub]
            hkk = "HK%d" % sub
            self.add("dve", lambda e: e.tensor_scalar(hk, cst[:, C_POW2:C_POW2 + 32], w0, None, ALU.mult), r=("consts", w0k), w=(hkk,))
            mid, midk = self.stat_slot()
            self.add("dve", lambda e: e.tensor_tensor(mid, mn, hk[:, 0:1], ALU.add), r=(mnk, hkk), w=(midk,))
            st[sub] = (mid, midk, hk, hkk, n, sk)

        def F2one(c, sub):
            los, st, kd = BS[c]
            if st[sub] is None or kd[sub] >= NB:
                return False
            k = kd[sub]
            kd[sub] = k + 1
            junkb = maskTs[c % 2].rearrange("p a b -> p (a b)")
            jk = "maskT%d" % (c % 2)
            mid, midk, hk, hkk, n, sk = st[sub]
            cnt, cntk = self.stat_slot()
            self.add("dve", lambda e: e.tensor_scalar(junkb[:, 0:n], score[sub][:, 0:n], mid, None, ALU.is_ge, ALU.add, accum_out=cnt),
                     r=(sk, midk), w=(jk, cntk))
            sg_, sgk = self.stat_slot()
            self.add("dve", lambda e: e.scalar_tensor_tensor(sg_, cnt, TOPK - 0.5, hk[:, k:k + 1], ALU.is_ge, ALU.mult),
                     r=(cntk, hkk), w=(sgk,))
            mid2, mid2k = self.stat_slot()
            self.add("dve", lambda e: e.scalar_tensor_tensor(mid2, mid, hk[:, k + 1:k + 2], sg_, ALU.subtract, ALU.add),
                     r=(midk, hkk, sgk), w=(mid2k,))
            st[sub] = (mid2, mid2k, hk, hkk, n, sk)
            return True

        def F2rem(c):
            los, st, kd = BS[c]
            return sum(NB - kd[s_] for s_ in range(2) if st[s_] is not None)

        def F2some(c, cnt_):
            los, st, kd = BS[c]
            for _ in range(cnt_):
                order = sorted(range(2), key=lambda s_: kd[s_])
                for s_ in order:
                    if F2one(c, s_):
                        break

        def F1(c):
            t0 = c * 256
            qT = qTs[c % 2]
            qTk = "qT%d" % (c % 2)
            for sub in range(2):
                self.dma(xt[sub][:, :], self.x[t0 + sub * 128:t0 + (sub + 1) * 128, :], r=(), w=("xt%d" % sub,),
                         semkey="xt%d" % sub)
                self.norm_to_hT(xt[sub], "xt%d" % sub, V_MIXPRE, hT, "hT", sub, hs, "hs")
            items = [(OFF_U + g * 128, ubuf[:, g, 16:272], "ubuf%d" % g) for g in range(4)]
            items += [(OFF_Q + h * 128, qT[:, h, :], qTk) for h in range(8)]
            items += [(OFF_K, KT[:, t0:t0 + 256], "KT%d" % c)]
            items += [(OFF_QI + j * 128, qiT[:, j, :], "qiT") for j in range(2)]
            for off, dst, dk in items:
                b = 4 + nxt("J", 2)
                for k in range(8):
                    self.mm(ps[:, b, 0:256], Wa[:, k, off:off + 128], hT[:, k, :], k == 0, k == 7, r=("Wa", "hT"), w=("ps%d" % b,))
                self.add("act", lambda e, dst=dst, b=b: e.activation(dst, ps[:, b, 0:256], AF.Copy), r=("ps%d" % b,), w=(dk,))
            b = 4 + nxt("J", 2)
            for half in range(2):
                for k in range(8):
                    self.mm(ps[half * 64:(half + 1) * 64, b, 0:256], Wa[:, k, OFF_KI:OFF_KI + 64], hT[:, k, :], k == 0, k == 7,
                            r=("Wa", "hT"), w=("ps%d" % b,))
            self.add("act", lambda e, b=b, t0=t0: e.activation(kiT[:, t0:t0 + 256], ps[:, b, 0:256], AF.Copy), r=("ps%d" % b,), w=("kiT%d" % c,))
            for sub in range(2):
                b = 4 + nxt("J", 2)
                for k in range(8):
                    self.mm(ps[:, b, 0:128], hT[:, k, sub * 128:(sub + 1) * 128], Wa[:, k, OFF_V:OFF_V + 128], k == 0, k == 7,
                            r=("Wa", "hT"), w=("ps%d" % b,))
                for k in range(8):
                    self.mm(ps[:, b, 128:132], hT[:, k, sub * 128:(sub + 1) * 128], Wa[:, k, OFF_WI:OFF_WI + 4], k == 0, k == 7,
                            r=("Wa", "hT"), w=("ps%d" % b,))
                qi = 2 * c + sub
                self.add("act", lambda e, b=b, qi=qi: e.activation(V[:, qi, 0:128], ps[:, b, 0:128], AF.Copy), r=("ps%d" % b,), w=("V%d" % c,))
                self.add("dve", lambda e, b=b, sub=sub: e.tensor_copy(widx[:, sub, :], ps[:, b, 128:132]), r=("ps%d" % b,), w=("widx",))
                idb = bass.AP(identb.tensor, 0, [[128, 128], [0, 4], [1, 128]])
                wb = bass.AP(widx.tensor, sub * 4, [[8, 128], [1, 4], [0, 128]])
                self.add("dve", lambda e, sub=sub, idb=idb, wb=wb: e.tensor_tensor(diagw[sub], idb, wb, ALU.mult),
                         r=("identb", "widx"), w=("diagw%d" % sub,))
            for g in range(4):
                uk = "ubuf%d" % g
                u = ubuf[:, g, :]
                bufs = [(pa, "pa"), (pb, "pb")]
                cur, curk, lo_ = u, uk, 0
                for lvl in range(g + 1):
                    sh = 1 << lvl
                    dstb, dstk = bufs[lvl % 2]
                    lo2 = lo_ + sh
                    self.add("pool", lambda e, dstb=dstb, cur=cur, lo2=lo2, sh=sh: e.tensor_tensor(
                        dstb[:, lo2:272], cur[:, lo2:272], cur[:, lo2 - sh:272 - sh], ALU.add), r=(curk,), w=(dstk,))
                    cur, curk, lo_ = dstb, dstk, lo2
                w = float(2 << g)
                self.add("pool", lambda e, cur=cur, w=w: e.tensor_scalar(
                    cur[:, 16:272], cur[:, 16:272], 1.0 / w, None, ALU.mult), r=(curk,), w=(curk,))
                self.add("pool", lambda e, cur=cur, u=u, g=g: e.tensor_tensor(
                    pooledT[:, g, :], cur[:, 16:272], u[:, 16:272], ALU.subtract), r=(curk, uk), w=("pooledT",))
                if c == 0:
                    self.add("pool", lambda e, cur=cur, g=g: e.tensor_tensor(
                        t16, cur[:, 16:32], cst[:, C_INVC + g * 16:C_INVC + (g + 1) * 16], ALU.mult), r=(curk, "consts"), w=("t16",))
                    self.add("pool", lambda e, w=w: e.tensor_scalar(t16, t16, w, None, ALU.mult), r=("t16",), w=("t16",))
                    self.add("pool", lambda e, u=u, g=g: e.tensor_tensor(pooledT[:, g, 0:16], t16, u[:, 16:32], ALU.subtract),
                             r=("t16", uk), w=("pooledT",))
                self.add("pool", lambda e, u=u: e.tensor_copy(u[:, 0:16], u[:, 256:272]), r=(uk,), w=(uk,))
            pm = pmT[c % 2]
            pmk = "pmT%d" % (c % 2)
            for g in range(4):
                b = 4 + nxt("J", 2)
                self.mm(ps[:, b, 0:256], Wpool[:, g, :], pooledT[:, g, :], True, True, r=("Wpool", "pooledT"), w=("ps%d" % b,))
                sc_ = self.vT[:, V_PSCALE * 8 + g:V_PSCALE * 8 + g + 1]
                self.add("dve", lambda e, g=g, b=b, sc_=sc_, pm=pm: e.tensor_scalar(pm[:, g, :], ps[:, b, 0:256], sc_, None, ALU.mult),
                         r=("ps%d" % b, "vT"), w=(pmk,))
            self.dma(self.PMd[:, :, t0:t0 + 256].rearrange("g p t -> p g t"), pm, r=(pmk,), w=(), semkey=pmk)
            jobs = []
            for sub in range(2):
                qi = 2 * c + sub
                n = 128 * (qi + 1)
                nch = (n + 511) // 512
                for kc in range(nch):
                    jobs.append((sub, n, kc, min(512, n - kc * 512), kc == nch - 1))
            jst = {}

            def dots(j):
                sub, n, kc, cols, last = jobs[j]
                ri = nxt("R", 2)
                R_ = Rb[ri]
                X = (0, 1, 2, 3) if nxt("S", 2) == 0 else (4, 5, 6, 7)
                jst[j] = (R_, ri, X)
                for h in range(4):
                    p0 = (h % 2) * 64
                    self.mm(ps[:, X[h], 0:cols], qiT[p0:p0 + 64, h // 2, sub * 128:(sub + 1) * 128],
                            kiT[p0:p0 + 64, kc * 512:kc * 512 + cols], True, True,
                            r=("qiT",) + tuple("kiT%d" % jj for jj in range(2 * kc, min(2 * kc + 2, c + 1))), w=("ps%d" % X[h],))
                    self.add("act", lambda e, R_=R_, h=h, cols=cols, X=X: e.activation(R_[:, h, 0:cols], ps[:, X[h], 0:cols], AF.Relu),
                             r=("ps%d" % X[h],), w=("R%d_%d" % (ri, h),))

            def smm(j):
                sub, n, kc, cols, last = jobs[j]
                R_, ri, X = jst[j]
                bS = X[0]
                for h in range(4):
                    self.mm(ps[:, bS, 0:cols], diagw[sub][:, h, :], R_[:, h, 0:cols], h == 0, (h == 3 and not last),
                            r=("diagw%d" % sub, "R%d_%d" % (ri, h)), w=("ps%d" % bS,))
                if last:
                    dc = n - 128 - kc * 512
                    self.mm(ps[:, bS, dc:dc + 128], identb, causb, False, True, r=("identb", "causb"), w=("ps%d" % bS,))
                self.add("act", lambda e, sub=sub, kc=kc, cols=cols, bS=bS: e.activation(
                    score[sub][:, kc * 512:kc * 512 + cols], ps[:, bS, 0:cols], AF.Copy), r=("ps%d" % bS,), w=("score%d" % sub,))

            los = [None, None]
            st = [None, None]
            kd = [0, 0]
            BS[c] = [los, st, kd]
            last0 = max(j for j in range(len(jobs)) if jobs[j][0] == 0)
            dots(0)
            for j in range(len(jobs)):
                if j + 1 < len(jobs):
                    dots(j + 1)
                smm(j)
                if j == last0:
                    bsetup(c, 0)
                elif j > last0 and st[0] is not None and kd[0] < NB - 2:
                    F2one(c, 0)
            bsetup(c, 1)

        def F2fin(c):
            los, st, kd = BS[c]
            for sub in range(2):
                if st[sub] is None:
                    continue
                mid, midk, hk, hkk, n, sk = st[sub]
                lo, lok = self.stat_slot()
                self.add("dve", lambda e, lo=lo, mid=mid, hk=hk: e.tensor_tensor(lo, mid, hk[:, NB:NB + 1], ALU.subtract),
                         r=(midk, hkk), w=(lok,))
                los[sub] = (lo, lok)

        def F3(c):
            los, st, kd = BS[c]
            maskT = maskTs[c % 2]
            mTk = "maskT%d" % (c % 2)
            for sub in range(2):
                qi = 2 * c + sub
                n = 128 * (qi + 1)
                nch = (n + 511) // 512
                lo, lok = los[sub]
                for kc in range(nch):
                    cols = min(512, n - kc * 512)
                    mi = nxt("T", 2)
                    mc = maskc[mi]
                    mck = "maskc%d" % mi
                    self.add("dve", lambda e, mc=mc, sub=sub, kc=kc, cols=cols, lo=lo: e.tensor_scalar(
                        mc[:, 0:cols], score[sub][:, kc * 512:kc * 512 + cols], lo, None, ALU.is_ge), r=("score%d" % sub, lok), w=(mck,))
                    bT = 6 + mi
                    nb4 = cols // 128
                    for b4 in range(nb4):
                        self.tr(ps[:, bT, b4 * 128:(b4 + 1) * 128], mc[:, b4 * 128:(b4 + 1) * 128], r=(mck,), w=("ps%d" % bT,))
                    src = ps[:, bT, 0:cols].rearrange("p (k t) -> p k t", k=nb4)
                    self.add("act", lambda e, src=src, kc=kc, nb4=nb4, sub=sub: e.activation(
                        maskT[:, kc * 4:kc * 4 + nb4, sub * 128:(sub + 1) * 128], src, AF.Copy), r=("ps%d" % bT,), w=(mTk,))

        def AH(c, h):
            qT = qTs[c % 2]
            qTk = "qT%d" % (c % 2)
            maskT = maskTs[c % 2]
            mTk = "maskT%d" % (c % 2)
            nkt = 2 * c + 2
            LA = 3
            bO = (4, 5) if h % 2 == 0 else (6, 7)
            slots = {}

            def front(kt, h=h):
                q0 = 0 if kt <= 2 * c else 128
                wq = 256 - q0
                bL, cL = nxt("L", 4), 0
                lk = "ps%d" % bL
                sl = nxt("P", 4)
                slots[kt] = (sl, q0)
                self.mm(ps[:, bL, cL:cL + wq], KT[:, kt * 128:(kt + 1) * 128], qT[:, h, q0:256], True, True,
                        r=("KT%d" % (kt // 2), qTk), w=(lk,))
                self.add("act", lambda e: e.activation(PT[sl][:, 0:wq], ps[:, bL, cL:cL + wq], AF.Exp, scale=SC),
                         r=(lk,), w=("PT%d" % sl,))
                me = "pool"
                self.add(me, lambda e: e.tensor_tensor(PTm[sl][:, 0:wq], PT[sl][:, 0:wq], maskT[:, kt, q0:256], ALU.mult),
                         r=("PT%d" % sl, mTk), w=("PTm%d" % sl,))
                eb = None
                if kt == 2 * c:
                    eb = (0, 256, 0)
                elif kt == 2 * c + 1:
                    eb = (0, 128, 0)
                elif kt == 2 * c - 1:
                    eb = (0, 128, 128)
                if eb is not None:
                    a0, a1, e0 = eb
                    self.add(me, lambda e: e.tensor_tensor(PTm[sl][:, a0:a1], PTm[sl][:, a0:a1], EB[:, h, e0:e0 + (a1 - a0)], ALU.mult),
                             r=("PTm%d" % sl, "EB"), w=("PTm%d" % sl,))

            def back(kt, h=h, bO=bO):
                sl, q0 = slots[kt]
                for s_ in range(2):
                    if kt > 2 * c + s_:
                        continue
                    c0 = s_ * 128 - q0
                    self.mm(ps[:, bO[s_], 0:129], PTm[sl][:, c0:c0 + 128], V[:, kt, 0:129], kt == 0, kt == 2 * c + s_,
                            r=("PTm%d" % sl, "V%d" % (kt // 2)), w=("ps%d" % bO[s_],))

            for kt in range(min(LA, nkt)):
                front(kt)
            for kt in range(nkt):
                back(kt)
                if kt + LA < nkt:
                    front(kt + LA)

        def AHN(c, h):
            bO = (4, 5) if h % 2 == 0 else (6, 7)
            for s_ in range(2):
                rd, rdk = self.stat_slot()
                self.add("dve", lambda e, rd=rd, b=bO[s_]: e.reciprocal(rd, ps[:, b, 128:129]), r=("ps%d" % bO[s_],), w=(rdk,))
                self.add("dve", lambda e, rd=rd, b=bO[s_], s_=s_, h=h: e.tensor_scalar(
                    att[s_][:, h * 128:(h + 1) * 128], ps[:, b, 0:128], rd, None, ALU.mult), r=("ps%d" % bO[s_], rdk), w=("att%d" % s_,))

        def AFN(c):
            t0 = c * 256
            at = atT[c % 2]
            atk = "atT%d" % (c % 2)
            for s_ in range(2):
                for half in range(2):
                    b = 6 + half
                    for hh in range(4):
                        h = half * 4 + hh
                        self.tr(ps[:, b, hh * 128:(hh + 1) * 128], att[s_][:, h * 128:(h + 1) * 128], r=("att%d" % s_,), w=("ps%d" % b,))
                    src = ps[:, b, :].rearrange("p (h t) -> p h t", h=4)
                    self.add("act", lambda e, src=src, half=half, s_=s_, at=at: e.activation(
                        at[:, half * 4:half * 4 + 4, s_ * 128:(s_ + 1) * 128], src, AF.Copy), r=("ps%d" % b,), w=(atk,))
            self.dma(self.ATd[:, :, t0:t0 + 256].rearrange("g p t -> p g t"), at, r=(atk,), w=(), semkey=atk)


        NCH = S // 256
        F1(0)
        F2some(0, F2rem(0))
        F2fin(0)
        F3(0)
        for c in range(NCH):
            nx = c + 1 < NCH
            if nx:
                F1(c + 1)
                rem = F2rem(c + 1)
            for h in range(8):
                if nx:
                    F2some(c + 1, ((h + 1) * rem) // 8 - (h * rem) // 8)
                AH(c, h)
                if h > 0:
                    AHN(c, h - 1)
            AHN(c, 7)
            if nx:
                F2fin(c + 1)
                F3(c + 1)
            AFN(c)

    def pass_B(self):
        self.common_alloc()
        ps = self.ps
        Wg = self.sb("Wg", [128, 8, 2048], BF16)
        Wpp = self.sb("Wpp", [128, 4, D], BF16)
        Wpa = self.sb("Wpa", [128, 8, D], BF16)
        Wout = self.sb("Wout", [128, 8, D], BF16)
        Wqm = self.sb("Wqm", [128, 8, 512], BF16)
        Wom = self.sb("Wom", [128, 4, D], BF16)
        Wkv = self.sb("Wkv", [128, 8, D], BF16)
        self.load_w("Wkv", Wkv, self.w_kv_mem, 8)
        self.load_w("Wg", Wg, self.w_in[:, OFF_GP:OFF_GP + 2048], 8)
        self.load_w("Wpp", Wpp, self.w_proj_pool, 4)
        self.load_w("Wpa", Wpa, self.w_proj_attn, 8)
        self.load_w("Wout", Wout, self.w_out, 8)
        self.load_w("Wqm", Wqm, self.w_q_mem, 8)
        self.load_w("Wom", Wom, self.w_o_mem, 4)
        gbc1 = self.gbc_load("gbc1", V_MIXPOST)
        gbc2 = self.gbc_load("gbc2", V_MEMPOST)
        xt = [self.sb("xt%d" % i, [128, D], F32) for i in range(4)]
        hs = self.sb("hs", [128, D], F32)
        tmp = self.sb("tmp", [128, D], F32)
        hTa = [self.sb("hTa%d" % i, [128, 8, 256], BF16) for i in range(2)]
        hTb = self.sb("hTb", [128, 8, 256], BF16)
        hT = hTb
        hTk = "hTb"
        pmT = [self.sb("pmT%d" % i, [128, 4, 256], BF16) for i in range(2)]
        atT = [self.sb("atT%d" % i, [128, 8, 256], BF16) for i in range(2)]
        mgT = self.sb("mgT", [128, 8, 256], BF16)
        sg = [self.sb("sg%d" % i, [128, 512], F32) for i in range(2)]
        mm_ = [self.sb("mm%d" % i, [128, 512], F32) for i in range(2)]
        memKT = self.sb("memKT", [128, 4, NMEM], BF16)
        Vm = self.sb("Vm", [128, 2, 4, 130], BF16)
        qmT = self.sb("qmT", [128, 4, 256], BF16)
        PmT = [self.sb("PmT%d" % i, [128, 2, 256], BF16) for i in range(2)]
        o = [self.sb("o%d" % i, [128, 512], F32) for i in range(2)]
        oT = self.sb("oT", [128, 4, 256], BF16)
        SC = float(128 ** -0.5)

        self.add("pool", lambda e: e.memset(Vm, 1.0), w=("Vm",))
        for mt in range(2):
            self.dma(xt[mt][:, :], self.mem[mt * 128:(mt + 1) * 128, :], r=(), w=("xt%d" % mt,), semkey="xt%d" % mt)
            self.norm_to_hT(xt[mt], "xt%d" % mt, V_MEMKV, hT, hTk, mt, hs, "hs")
        for h in range(4):
            b = h % 2
            for k in range(8):
                self.mm(ps[:, b, 0:256], Wkv[:, k, h * 128:(h + 1) * 128], hT[:, k, :], k == 0, k == 7,
                        r=("Wkv", hTk), w=("ps%d" % b,))
            self.add("act", lambda e, h=h, b=b: e.activation(memKT[:, h, :], ps[:, b, 0:256], AF.Copy),
                     r=("ps%d" % b,), w=("memKT",))
        for mt in range(2):
            b = 2 + mt
            for k in range(8):
                self.mm(ps[:, b, :], hT[:, k, mt * 128:(mt + 1) * 128], Wkv[:, k, 512:1024], k == 0, k == 7,
                        r=("Wkv", hTk), w=("ps%d" % b,))
            src = ps[:, b, :].rearrange("p (h d) -> p h d", h=4)
            self.add("act", lambda e, mt=mt, src=src: e.activation(Vm[:, mt, :, 0:128], src, AF.Copy),
                     r=("ps%d" % b,), w=("Vm",))

        def load_norm(c):
            t0 = c * 256
            pm = pmT[c % 2]
            pmk = "pmT%d" % (c % 2)
            at = atT[c % 2]
            atk = "atT%d" % (c % 2)
            hT = hTa[c % 2]
            hTk = "hTa%d" % (c % 2)
            xs = [(xt[(c % 2) * 2 + sub], "xt%d" % ((c % 2) * 2 + sub)) for sub in range(2)]
            self.dma(pm, self.PMd[:, :, t0:t0 + 256].rearrange("g p t -> p g t"), r=(), w=(pmk,), semkey=pmk)
            self.dma(at, self.ATd[:, :, t0:t0 + 256].rearrange("g p t -> p g t"), r=(), w=(atk,), semkey=atk)
            for sub in range(2):
                self.dma(xs[sub][0][:, :], self.x[t0 + sub * 128:t0 + (sub + 1) * 128, :], r=(), w=(xs[sub][1],), semkey=xs[sub][1])
                self.norm_to_hT(xs[sub][0], xs[sub][1], V_MIXPRE, hT, hTk, sub, hs, "hs", banks=(2, 3))

        def merge_out(c):
            t0 = c * 256
            pm = pmT[c % 2]
            pmk = "pmT%d" % (c % 2)
            at = atT[c % 2]
            atk = "atT%d" % (c % 2)
            hT = hTa[c % 2]
            hTk = "hTa%d" % (c % 2)
            xs = [(xt[(c % 2) * 2 + sub], "xt%d" % ((c % 2) * 2 + sub)) for sub in range(2)]
            for j in range(8):
                bA = (2 * j) % 4
                bB = bA + 1
                for k in range(8):
                    self.mm(ps[:, bA, 0:256], Wg[:, k, j * 128:(j + 1) * 128], hT[:, k, :], k == 0, k == 7,
                            r=("Wg", hTk), w=("ps%d" % bA,))
                for k in range(8):
                    self.mm(ps[:, bA, 256:512], Wg[:, k, 1024 + j * 128:1024 + (j + 1) * 128], hT[:, k, :], k == 0, k == 7,
                            r=("Wg", hTk), w=("ps%d" % bA,))
                for k in range(4):
                    self.mm(ps[:, bB, 0:256], Wpp[:, k, j * 128:(j + 1) * 128], pm[:, k, :], k == 0, k == 3,
                            r=("Wpp", pmk), w=("ps%d" % bB,))
                for k in range(8):
                    self.mm(ps[:, bB, 256:512], Wpa[:, k, j * 128:(j + 1) * 128], at[:, k, :], k == 0, k == 7,
                            r=("Wpa", atk), w=("ps%d" % bB,))
                s_ = sg[j % 2]
                sk = "sg%d" % (j % 2)
                m_ = mm_[j % 2]
                mk = "mm%d" % (j % 2)
                self.add("act", lambda e, s_=s_, bA=bA: e.activation(s_, ps[:, bA, :], AF.Sigmoid), r=("ps%d" % bA,), w=(sk,))
                self.add("dve", lambda e, s_=s_, m_=m_, bB=bB: e.tensor_tensor(m_, s_, ps[:, bB, :], ALU.mult),
                         r=(sk, "ps%d" % bB), w=(mk,))
                self.add("pool", lambda e, m_=m_, j=j: e.tensor_tensor(mgT[:, j, :], m_[:, 0:256], m_[:, 256:512], ALU.add),
                         r=(mk,), w=("mgT",))
            for sub in range(2):
                yb = (4, 5) if sub == 0 else (6, 7)
                for half in range(2):
                    for k in range(8):
                        self.mm(ps[:, yb[half], :], mgT[:, k, sub * 128:(sub + 1) * 128], Wout[:, k, half * 512:(half + 1) * 512],
                                k == 0, k == 7, r=("mgT", "Wout"), w=("ps%d" % yb[half],))
                self.post_norm_residual(yb, gbc1, "gbc1", xs[sub][0], xs[sub][1], tmp, "tmp")

        def memattn(c):
            t0 = c * 256
            pm = pmT[c % 2]
            pmk = "pmT%d" % (c % 2)
            at = atT[c % 2]
            atk = "atT%d" % (c % 2)
            hT = hTa[c % 2]
            hTk = "hTa%d" % (c % 2)
            xs = [(xt[(c % 2) * 2 + sub], "xt%d" % ((c % 2) * 2 + sub)) for sub in range(2)]
            hT = hTb
            hTk = "hTb"
            for sub in range(2):
                self.norm_to_hT(xs[sub][0], xs[sub][1], V_MEMPRE, hT, hTk, sub, hs, "hs", banks=(0, 1))
            for h in range(4):
                b = 2 + h % 2
                for k in range(8):
                    self.mm(ps[:, b, 0:256], Wqm[:, k, h * 128:(h + 1) * 128], hT[:, k, :], k == 0, k == 7,
                            r=("Wqm", hTk), w=("ps%d" % b,))
                self.add("act", lambda e, h=h, b=b: e.activation(qmT[:, h, :], ps[:, b, 0:256], AF.Copy),
                         r=("ps%d" % b,), w=("qmT",))
            for h in range(4):
                bL = h % 2
                P_ = PmT[h % 2]
                Pk = "PmT%d" % (h % 2)
                for mt in range(2):
                    self.mm(ps[:, bL, mt * 256:(mt + 1) * 256], memKT[:, h, mt * 128:(mt + 1) * 128], qmT[:, h, :], True, True,
                            r=("memKT", "qmT"), w=("ps%d" % bL,))
                src = ps[:, bL, :].rearrange("p (m t) -> p m t", m=2)
                self.add("act", lambda e, P_=P_, src=src: e.activation(P_, src, AF.Exp, scale=SC), r=("ps%d" % bL,), w=(Pk,))
                for sub in range(2):
                    bO = 4 + sub * 2 + (h // 3)
                    col = (h % 3) * 130
                    for mt in range(2):
                        self.mm(ps[:, bO, col:col + 129], P_[:, mt, sub * 128:(sub + 1) * 128], Vm[:, mt, h, 0:129],
                                mt == 0, mt == 1, r=(Pk, "Vm"), w=("ps%d" % bO,))
                    rd, rdk = self.stat_slot()
                    self.add("dve", lambda e, rd=rd, bO=bO, col=col: e.reciprocal(rd, ps[:, bO, col + 128:col + 129]),
                             r=("ps%d" % bO,), w=(rdk,))
                    self.add("dve", lambda e, rd=rd, bO=bO, col=col, sub=sub, h=h: e.tensor_scalar(
                        o[sub][:, h * 128:(h + 1) * 128], ps[:, bO, col:col + 128], rd, None, ALU.mult),
                        r=("ps%d" % bO, rdk), w=("o%d" % sub,))
            for sub in range(2):
                b = sub
                for h in range(4):
                    self.tr(ps[:, b, h * 128:(h + 1) * 128], o[sub][:, h * 128:(h + 1) * 128], r=("o%d" % sub,), w=("ps%d" % b,))
                src = ps[:, b, :].rearrange("p (h t) -> p h t", h=4)
                self.add("act", lambda e, src=src, sub=sub: e.activation(oT[:, :, sub * 128:(sub + 1) * 128], src, AF.Copy),
                         r=("ps%d" % b,), w=("oT",))
            for sub in range(2):
                yb = (4, 5) if sub == 0 else (6, 7)
                for half in range(2):
                    for k in range(4):
                        self.mm(ps[:, yb[half], :], oT[:, k, sub * 128:(sub + 1) * 128], Wom[:, k, half * 512:(half + 1) * 512],
                                k == 0, k == 3, r=("oT", "Wom"), w=("ps%d" % yb[half],))
                self.post_norm_residual(yb, gbc2, "gbc2", xs[sub][0], xs[sub][1], tmp, "tmp")
                dst = self.X2d if "C" in self.stages or True else self.out
                self.dma(dst[t0 + sub * 128:t0 + (sub + 1) * 128, :], xs[sub][0][:, :], r=(xs[sub][1],), w=(),
                         semkey=xs[sub][1])


        NCH = S // 256
        load_norm(0)
        for c in range(NCH):
            merge_out(c)
            if c + 1 < NCH:
                load_norm(c + 1)
            memattn(c)
    def build(self):
        self.bodies = []
        if "A" in self.stages:
            self.bodies.append(self.pass_A)
        if "B" in self.stages:
            self.bodies.append(self.pass_B)
        if "C" in self.stages:
            self.bodies.append(self.pass_C)
        for b in self.bodies:
            self.run_pass(b)
        return self.nc


def make_in_maps(inputs):
    f = lambda a: np.ascontiguousarray(np.asarray(a, dtype=np.float32))
    vec_names = ["norm_mix_pre", "norm_mix_post", "norm_mem_pre", "norm_mem_kv", "norm_mem_post",
                 "norm_ffn_pre", "norm_ffn_post"]
    vecs = np.zeros((8, D), np.float32)
    for i, n in enumerate(vec_names):
        vecs[i] = f(inputs[n])
    vecs[V_PSCALE, :512] = f(inputs["pool_scale"])
    vecsT = np.ascontiguousarray(vecs.reshape(8, 8, 128).transpose(2, 0, 1).reshape(128, 64))
    shared = {k: f(inputs[k]) for k in ["w_in", "pool_w", "w_proj_pool", "w_proj_attn", "rel_bias_table", "w_out",
                                        "w_q_mem", "w_kv_mem", "w_o_mem", "w_gate_up", "w_down"]}
    shared["vecs"] = vecs
    shared["vecsT"] = vecsT
    shared["consts"] = make_consts()
    x = f(inputs["x"])
    mem = f(inputs["mem"])
    maps = []
    for b in range(x.shape[0]):
        m = dict(shared)
        m["x"] = x[b]
        m["mem"] = mem[b]
        maps.append(m)
    return maps


_NC_CACHE = {}


def kernel(**inputs):
    if "full" not in _NC_CACHE:
        _NC_CACHE["full"] = Builder("ABC").build()
    nc = _NC_CACHE["full"]
    maps = make_in_maps(inputs)
    res = run_bass_kernel_spmd(nc, maps, core_ids=list(range(len(maps))))
    return np.stack([np.asarray(r["out"], dtype=np.float32) for r in res.results], axis=0)
```
